# Optimizing a Trainium2 kernel written in Bass

```python
import jax, jax.numpy as jnp
from jax import lax
import numpy as np

D_MODEL = 1024
BATCH = 32
SEQ = 2048
DEPTH = 4

N_MIXERS = 2
N_GLA_LAYERS = (DEPTH + 1) // 2
N_SB_LAYERS = DEPTH // 2
GLA_HEADS = 4
GLA_KD = D_MODEL // 2
GLA_VD = D_MODEL
GLA_HK = GLA_KD // GLA_HEADS
GLA_HV = GLA_VD // GLA_HEADS
GLA_GATE_RANK = 16
GLA_TAU = 16.0
GLA_CHUNK = 64
GLA_IN = 2 * GLA_KD + 2 * GLA_VD + GLA_GATE_RANK
SB_HEAD_DIM = 64
SB_HEADS = D_MODEL // SB_HEAD_DIM
SB_QBLOCK = 128
D_FF = ((8 * D_MODEL // 3 + 255) // 256) * 256
NORM_EPS = 1e-6

kernel_name = 'hybrid_gla_stickbreaking_swiglu'


def _rmsnorm(x, g):
    xf = x.astype(jnp.float32)
    y = xf * lax.rsqrt(jnp.mean(xf * xf, axis=-1, keepdims=True) + NORM_EPS)
    return (y * g.astype(jnp.float32)).astype(x.dtype)


def _gla_mixer(h, w_in, w_gate2, b_gate, norm_g, w_out):
    B, S, _ = h.shape
    n_chunks = S // GLA_CHUNK
    proj = h @ w_in
    q, k, v, r, g_lr = jnp.split(proj, [GLA_KD, 2 * GLA_KD, 2 * GLA_KD + GLA_VD, 2 * GLA_KD + 2 * GLA_VD], axis=-1)
    log_a = jax.nn.log_sigmoid((g_lr @ w_gate2 + b_gate).astype(jnp.float32)) / GLA_TAU

    def to_chunks(t, d):
        return t.astype(jnp.float32).reshape(B, n_chunks, GLA_CHUNK, GLA_HEADS, d).transpose(1, 0, 3, 2, 4)

    qc = to_chunks(q, GLA_HK) * (GLA_HK ** -0.5)
    kc = to_chunks(k, GLA_HK)
    vc = to_chunks(v, GLA_HV)
    ac = to_chunks(log_a, GLA_HK)
    causal = jnp.tril(jnp.ones((GLA_CHUNK, GLA_CHUNK), dtype=bool))

    def step(state, inp):
        qb, kb, vb, ab = inp
        bcum = jnp.cumsum(ab, axis=2)
        diff = bcum[:, :, :, None, :] - bcum[:, :, None, :, :]
        decay = jnp.exp(jnp.where(causal[:, :, None], diff, -jnp.inf))
        scores = jnp.einsum('bhtk,bhsk,bhtsk->bhts', qb, kb, decay)
        o = jnp.einsum('bhts,bhsv->bhtv', scores, vb) + jnp.einsum('bhtk,bhkv->bhtv', qb * jnp.exp(bcum), state)
        blast = bcum[:, :, -1:, :]
        state = state * jnp.exp(blast[:, :, 0, :, None]) + jnp.einsum('bhsk,bhsv->bhkv', kb * jnp.exp(blast - bcum), vb)
        return state, o

    state0 = jnp.zeros((B, GLA_HEADS, GLA_HK, GLA_HV), jnp.float32)
    _, o = lax.scan(step, state0, (qc, kc, vc, ac))
    o = o.transpose(1, 0, 3, 2, 4).reshape(B, S, GLA_HEADS, GLA_HV)
    o = _rmsnorm(o, norm_g).reshape(B, S, GLA_VD)
    o = o * jax.nn.silu(r.astype(jnp.float32))
    return o.astype(h.dtype) @ w_out


def _sb_mixer(h, w_in, q_norm_g, k_norm_g, w_out):
    B, S, _ = h.shape
    q, k, v = jnp.split(h @ w_in, 3, axis=-1)
    heads = lambda t: t.reshape(B, S, SB_HEADS, SB_HEAD_DIM).transpose(0, 2, 1, 3)
    q = _rmsnorm(heads(q), q_norm_g).astype(jnp.float32)
    k = _rmsnorm(heads(k), k_norm_g).astype(jnp.float32)
    v = heads(v).astype(jnp.float32)
    scale = SB_HEAD_DIM ** -0.5
    outs = []
    for blk in range(S // SB_QBLOCK):
        t0 = blk * SB_QBLOCK
        t1 = t0 + SB_QBLOCK
        kb = k[:, :, :t1]
        vb = v[:, :, :t1]
        z = jnp.einsum('bhtd,bhsd->bhts', q[:, :, t0:t1], kb) * scale
        t_idx = t0 + jnp.arange(SB_QBLOCK)[:, None]
        s_idx = jnp.arange(t1)[None, :]
        mask = s_idx < t_idx
        log_1mb = jnp.where(mask, jax.nn.log_sigmoid(-z), 0.0)
        rem = lax.cumsum(log_1mb, axis=3, reverse=True) - log_1mb
        w = jnp.where(mask, jnp.exp(jax.nn.log_sigmoid(z) + rem), 0.0)
        outs.append(jnp.einsum('bhts,bhsd->bhtd', w, vb))
    o = jnp.concatenate(outs, axis=2).transpose(0, 2, 1, 3).reshape(B, S, D_MODEL)
    return o.astype(h.dtype) @ w_out


def _swiglu(h, w_gate_up, w_down):
    g, u = jnp.split(h @ w_gate_up, 2, axis=-1)
    return (jax.nn.silu(g) * u) @ w_down


def setup_inputs(seed: int = 0) -> dict:
    key = jax.random.key(seed)
    ks = jax.random.split(key, 16)
    nrm = lambda k, shape, fan_in: jax.random.normal(k, shape, jnp.float32) * (fan_in ** -0.5)
    gain = lambda k, shape: 1.0 + 0.02 * jax.random.normal(k, shape, jnp.float32)
    return {
        'x': jax.random.normal(ks[0], (BATCH, SEQ, D_MODEL), jnp.float32),
        'attn_norm_g': gain(ks[1], (DEPTH, D_MODEL)),
        'ffn_norm_g': gain(ks[2], (DEPTH, D_MODEL)),
        'gla_w_in': nrm(ks[3], (N_GLA_LAYERS, D_MODEL, GLA_IN), D_MODEL),
        'gla_w_gate2': nrm(ks[4], (N_GLA_LAYERS, GLA_GATE_RANK, GLA_KD), GLA_GATE_RANK),
        'gla_b_gate': 0.01 * jax.random.normal(ks[5], (N_GLA_LAYERS, GLA_KD), jnp.float32),
        'gla_norm_g': gain(ks[6], (N_GLA_LAYERS, GLA_HV)),
        'gla_w_out': nrm(ks[7], (N_GLA_LAYERS, GLA_VD, D_MODEL), GLA_VD),
        'sb_w_in': nrm(ks[8], (N_SB_LAYERS, D_MODEL, 3 * D_MODEL), D_MODEL),
        'sb_q_norm_g': gain(ks[9], (N_SB_LAYERS, SB_HEAD_DIM)),
        'sb_k_norm_g': gain(ks[10], (N_SB_LAYERS, SB_HEAD_DIM)),
        'sb_w_out': nrm(ks[11], (N_SB_LAYERS, D_MODEL, D_MODEL), D_MODEL),
        'ffn_w_gate_up': nrm(ks[12], (DEPTH, D_MODEL, 2 * D_FF), D_MODEL),
        'ffn_w_down': nrm(ks[13], (DEPTH, D_FF, D_MODEL), D_FF),
    }


def reference(x, attn_norm_g, ffn_norm_g, gla_w_in, gla_w_gate2, gla_b_gate, gla_norm_g, gla_w_out,
              sb_w_in, sb_q_norm_g, sb_k_norm_g, sb_w_out, ffn_w_gate_up, ffn_w_down):
    for i in range(DEPTH):
        h = _rmsnorm(x, attn_norm_g[i])
        j = i // N_MIXERS
        if i % N_MIXERS == 0:
            x = x + _gla_mixer(h, gla_w_in[j], gla_w_gate2[j], gla_b_gate[j], gla_norm_g[j], gla_w_out[j])
        else:
            x = x + _sb_mixer(h, sb_w_in[j], sb_q_norm_g[j], sb_k_norm_g[j], sb_w_out[j])
        h = _rmsnorm(x, ffn_norm_g[i])
        x = x + _swiglu(h, ffn_w_gate_up[i], ffn_w_down[i])
    return x
```

```python
import contextlib
import numpy as np
import concourse.bass as bass
import concourse.mybir as mybir
from concourse.bass_utils import run_bass_kernel_spmd

F32 = mybir.dt.float32
BF16 = mybir.dt.bfloat16
AF = mybir.ActivationFunctionType
ALU = mybir.AluOpType

D = 1024
S = 2048
DFF = 2816
KD = 512
GIN = 3088
DEPTH = 4
NCORES = 8
EPS = 1e-6


class Prog:
    def __init__(self, nc):
        self.nc = nc
        self.ops = []
        self.lw = {}
        self.rd = {}
        self.last_bar = {}
        self.bar_dmas = []
        self.bar_deps = set()
        self.bar_pending = set()
        self.const_keys = []
        self.const_done = set()

    def barrier(self):
        self.bar_deps = set(self.last_bar.values()) | set(self.bar_dmas)
        self.bar_dmas = []
        self.bar_pending = {'pe', 'act', 'dve', 'pool', 'sp'}

    def op(self, eng, fn, reads=(), writes=(), creads=(), dma=None, bar=None):
        if bar is None:
            bar = dma is None
        raw = set()
        oth = set()
        lw = self.lw
        for k in reads:
            w = lw.get(k)
            if w is not None:
                raw.add(w)
        for k in creads:
            w = lw.get(k)
            if w is not None:
                raw.add(w)
        for k in writes:
            w = lw.get(k)
            if w is not None:
                oth.add(w)
            r = self.rd.get(k)
            if r:
                oth.update(r.values())
        i = len(self.ops)
        deps = set()
        ops = self.ops
        for d in raw:
            de = ops[d]
            if de[3] is None and dma is None and de[0] == eng and eng == 'pe':
                continue
            deps.add(d)
        for d in oth:
            de = ops[d]
            if de[3] is None and dma is None and de[0] == eng and eng == 'pe':
                continue
            deps.add(d)
        if dma is None and self.const_keys and eng not in self.const_done:
            self.const_done.add(eng)
            for k in self.const_keys:
                w = lw.get(k)
                if w is not None and not (ops[w][3] is None and ops[w][0] == eng and eng == 'pe'):
                    deps.add(w)
        if bar and eng in self.bar_pending:
            self.bar_pending.discard(eng)
            for d in self.bar_deps:
                de = ops[d]
                if de[3] is None and dma is None and de[0] == eng and eng == 'pe':
                    continue
                deps.add(d)
        ops.append((eng, fn, deps, dma))
        for k in reads:
            self.rd.setdefault(k, {})[(eng, i if dma is not None else -1)] = i
        for k in writes:
            lw[k] = i
            self.rd[k] = {}
        if bar:
            if dma is None:
                self.last_bar[eng] = i
            else:
                self.bar_dmas.append(i)
        return i

    def emit(self, final_wait_ops=()):
        nc = self.nc
        ops = self.ops
        n = len(ops)
        needed = [False] * n
        for (eng, fn, deps, dma) in ops:
            for d in deps:
                needed[d] = True
        for d in final_wait_ops:
            needed[d] = True
        stack = contextlib.ExitStack()
        engsem = {}
        for e in ('pe', 'act', 'dve', 'pool', 'sp'):
            engsem[e] = stack.enter_context(nc.semaphore('s_' + e))
        dmasem = {}
        for (eng, fn, deps, dma) in ops:
            if dma is not None and dma not in dmasem:
                dmasem[dma] = stack.enter_context(nc.semaphore('d_%d' % len(dmasem)))
        tok = [None] * n
        cnt = {}
        for i, (eng, fn, deps, dma) in enumerate(ops):
            if dma is not None:
                cnt[dma] = cnt.get(dma, 0) + 16
                tok[i] = (dmasem[dma], cnt[dma], dma)
            elif needed[i]:
                cnt[eng] = cnt.get(eng, 0) + 1
                tok[i] = (engsem[eng], cnt[eng], eng)
        self.maxcnt = dict(cnt)
        per = {e: [] for e in ('pe', 'act', 'dve', 'pool', 'sp')}
        for i, o in enumerate(ops):
            per[o[0]].append(i)
        fw = list(final_wait_ops)

        def run(ename, e):
            waited = {}
            for i in per[ename]:
                eng, fn, deps, dma = ops[i]
                for d in sorted(deps):
                    s, v, key = tok[d]
                    if waited.get(key, 0) < v:
                        e.wait_ge(s, v)
                        waited[key] = v
                ins = fn(e)
                if dma is not None:
                    ins.then_inc(tok[i][0], 16)
                elif needed[i]:
                    ins.then_inc(tok[i][0], 1)
            if ename == 'sp':
                for d in fw:
                    s, v, key = tok[d]
                    if waited.get(key, 0) < v:
                        e.wait_ge(s, v)
                        waited[key] = v

        with stack:
            with nc.Block() as block:
                @block.tensor
                def _(e):
                    run('pe', e)

                @block.scalar
                def _(e):
                    run('act', e)

                @block.vector
                def _(e):
                    run('dve', e)

                @block.gpsimd
                def _(e):
                    run('pool', e)

                @block.sync
                def _(e):
                    run('sp', e)


ARENA = 74 * 1024
NS = 2
NB = 3
WCAP = 2048


def build(nseq, layers):
    nc = bass.Bass("TRN2", target_bir_lowering=False)
    P = Prog(nc)

    def din(name, shape):
        return nc.dram_tensor(name, list(shape), F32, kind="ExternalInput").ap()

    x_d = din("x", [nseq, S, D])
    attn_g_d = din("attn_norm_g", [DEPTH, D])
    ffn_g_d = din("ffn_norm_g", [DEPTH, D])
    gla_w_in_d = din("gla_w_in", [2, D, GIN])
    gla_w2_d = din("gla_w_gate2", [2, 16, KD])
    gla_b_d = din("gla_b_gate", [2, KD])
    gla_ng_d = din("gla_norm_g", [2, 256])
    gla_wo_d = din("gla_w_out", [2, D, D])
    sb_w_in_d = din("sb_w_in", [2, D, 3 * D])
    sb_qg_d = din("sb_q_norm_g", [2, 64])
    sb_kg_d = din("sb_k_norm_g", [2, 64])
    sb_wo_d = din("sb_w_out", [2, D, D])
    ffn_wgu_d = din("ffn_w_gate_up", [DEPTH, D, 2 * DFF])
    ffn_wd_d = din("ffn_w_down", [DEPTH, DFF, D])
    y_d = nc.dram_tensor("y", [nseq, S, D], F32, kind="ExternalOutput").ap()

    xT = nc.alloc_sbuf_tensor("xT", [128, 8, S], F32)
    hT = nc.alloc_sbuf_tensor("hT", [128, 8, S], BF16)
    wst = [nc.alloc_sbuf_tensor("wst%d" % i, [128, WCAP], F32) for i in range(NS)]
    wbf = [nc.alloc_sbuf_tensor("wbf%d" % i, [128, WCAP], BF16) for i in range(NB)]
    arena = nc.alloc_sbuf_tensor("arena", [128, ARENA // 2], BF16)
    ident_f = nc.alloc_sbuf_tensor("ident_f", [128, 128], F32)
    ident_b = nc.alloc_sbuf_tensor("ident_b", [128, 128], BF16)
    ones_b = nc.alloc_sbuf_tensor("ones_b", [128, 128], BF16)
    blk_b = nc.alloc_sbuf_tensor("blk_b", [128, 128], BF16)
    negU_b = nc.alloc_sbuf_tensor("negU_b", [128, 128], BF16)
    negO_b = nc.alloc_sbuf_tensor("negO_b", [128, 128], BF16)
    negbig_b = nc.alloc_sbuf_tensor("negbig_b", [128, 128], BF16)
    mask01_b = nc.alloc_sbuf_tensor("mask01_b", [128, 128], BF16)
    zeros_b = nc.alloc_sbuf_tensor("zeros_b", [128, 512], BF16)
    triS_f = nc.alloc_sbuf_tensor("triS_f", [128, 128], F32)
    triR_f = nc.alloc_sbuf_tensor("triR_f", [128, 128], F32)
    maskG_b = nc.alloc_sbuf_tensor("maskG_b", [128, 4, 128], BF16)
    onecol = nc.alloc_sbuf_tensor("onecol", [128, 1], F32)
    epscol = nc.alloc_sbuf_tensor("epscol", [128, 1], F32)
    gattn = nc.alloc_sbuf_tensor("gattn", [128, DEPTH, 8], F32)
    gffn = nc.alloc_sbuf_tensor("gffn", [128, DEPTH, 8], F32)
    gq = nc.alloc_sbuf_tensor("gq", [128, 2], F32)
    gk = nc.alloc_sbuf_tensor("gk", [128, 2], F32)
    ones_f = nc.alloc_sbuf_tensor("ones_f", [1, 128], F32)
    W2s = nc.alloc_sbuf_tensor("W2s", [32, 512], F32)
    grow = nc.alloc_sbuf_tensor("grow", [1, 256], F32)
    PS = [nc.alloc_psum_tensor("ps%d" % i, [128, 512], F32) for i in range(8)]

    def pk(i):
        return ('ps', i)

    def aview(off, shape, dt):
        nel = 1
        for s_ in shape[1:]:
            nel *= s_
        nb = nel * (4 if dt == F32 else 2)
        assert off % 4 == 0 and off + nb <= ARENA, (off, nb)
        ap = arena[:, off // 2:(off + nb) // 2]
        if dt == F32:
            ap = ap.bitcast(F32)
        if len(shape) == 3:
            ap = ap.rearrange("p (a b) -> p a b", b=shape[2])
        elif len(shape) == 4:
            ap = ap.rearrange("p (a b c) -> p a b c", b=shape[2], c=shape[3])
        if shape[0] != 128:
            ap = ap[0:shape[0]]
        return ap

    def MM(out, lhsT, rhs, start, stop, r, w, cr=()):
        P.op('pe', lambda e: e.matmul(out, lhsT, rhs, start=start, stop=stop, skip_group_check=True), reads=r, writes=w, creads=cr)

    def TR(out, in_, ident, r, w):
        P.op('pe', lambda e: e.transpose(out, in_, ident), reads=r, writes=w)

    def ACT(out, in_, func, r, w, scale=None, bias=None, accum_out=None):
        kw = {}
        if scale is not None:
            kw['scale'] = scale
        if bias is not None:
            kw['bias'] = bias
        if accum_out is not None:
            kw['accum_out'] = accum_out
        P.op('act', lambda e: e.activation(out, in_, func, **kw), reads=r, writes=w)

    def TT(eng, out, in0, in1, op, r, w):
        P.op(eng, lambda e: e.tensor_tensor(out, in0, in1, op), reads=r, writes=w)

    def STT(out, in0, scalar, in1, op0, op1, r, w):
        P.op('dve', lambda e: e.scalar_tensor_tensor(out, in0, scalar, in1, op0, op1), reads=r, writes=w)

    def CP(eng, out, in_, r, w, bar=None):
        if eng == 'act':
            P.op('act', lambda e: e.activation(out, in_, AF.Copy), reads=r, writes=w, bar=bar)
        else:
            P.op(eng, lambda e: e.tensor_copy(out, in_), reads=r, writes=w, bar=bar)

    def MS(eng, ap, val, w, r=()):
        P.op(eng, lambda e: e.memset(ap, val), reads=r, writes=w)

    def ASEL(ap, pattern, cmp, fill, base, cm, key):
        P.op('pool', lambda e: e.affine_select(ap, ap, pattern, cmp, fill, base=base, channel_multiplier=cm), reads=[key], writes=[key])

    def DMA(out, in_, key, r=(), w=(), eng='sp', bar=False, slow=False):
        if slow:
            return P.op(eng, lambda e: e.dma_start(out=out, in_=in_, allow_slow_non_contiguous=True), reads=r, writes=w, dma=key, bar=bar)
        return P.op(eng, lambda e: e.dma_start(out=out, in_=in_), reads=r, writes=w, dma=key, bar=bar)

    MS('pool', ident_f[:], 1.0, ['ident_f'])
    ASEL(ident_f[:], [[-1, 128]], ALU.is_equal, 0.0, 0, 1, 'ident_f')
    MS('pool', ident_b[:], 1.0, ['ident_b'])
    ASEL(ident_b[:], [[-1, 128]], ALU.is_equal, 0.0, 0, 1, 'ident_b')
    MS('pool', ones_b[:], 1.0, ['ones_b'])
    MS('pool', blk_b[:], 1.0, ['blk_b'])
    MS('pool', blk_b[0:64, 64:128], 0.0, ['blk_b'])
    MS('pool', blk_b[64:128, 0:64], 0.0, ['blk_b'])
    MS('pool', negU_b[:], -1.0, ['negU_b'])
    ASEL(negU_b[:], [[-1, 128]], ALU.is_ge, 0.0, 0, 1, 'negU_b')
    MS('pool', negO_b[:], -1.0, ['negO_b'])
    MS('pool', negbig_b[:], -30000.0, ['negbig_b'])
    ASEL(negbig_b[:], [[-1, 128]], ALU.is_ge, 0.0, 0, 1, 'negbig_b')
    MS('pool', mask01_b[:], 1.0, ['mask01_b'])
    ASEL(mask01_b[:], [[1, 128]], ALU.is_gt, 0.0, 0, -1, 'mask01_b')
    MS('pool', zeros_b[:], 0.0, ['zeros_b'])
    MS('pool', triS_f[:], -1.0 / 16.0, ['triS_f'])
    ASEL(triS_f[:], [[1, 128]], ALU.is_ge, 0.0, 0, -1, 'triS_f')
    MS('pool', triS_f[0:64, 64:128], 0.0, ['triS_f'])
    MS('pool', triR_f[:], -1.0 / 16.0, ['triR_f'])
    ASEL(triR_f[:], [[-1, 128]], ALU.is_gt, 0.0, 0, 1, 'triR_f')
    MS('pool', triR_f[64:128, 0:64], 0.0, ['triR_f'])
    MS('pool', maskG_b[:], 1.0, ['maskG_b'])
    ASEL(maskG_b[:], [[0, 4], [1, 128]], ALU.is_ge, 0.0, 0, -1, 'maskG_b')
    MS('pool', maskG_b[0:64, :, 64:128], 0.0, ['maskG_b'])
    MS('pool', onecol[:], 1.0, ['onecol'])
    MS('pool', epscol[:], EPS, ['epscol'])
    MS('pool', ones_f[:], 1.0, ['ones_f'])
    CONSTS = ['ident_f', 'ident_b', 'ones_b', 'blk_b', 'negU_b', 'negO_b', 'negbig_b', 'mask01_b', 'zeros_b',
              'triS_f', 'triR_f', 'maskG_b', 'onecol', 'epscol', 'ones_f', 'gattn', 'gffn', 'gq', 'gk']

    DMA(gattn[:], attn_g_d.rearrange("l (c p) -> p l c", p=128), 'gattn', w=['gattn'], eng='act', slow=True)
    DMA(gffn[:], ffn_g_d.rearrange("l (c p) -> p l c", p=128), 'gffn', w=['gffn'], eng='act', slow=True)
    for j in range(2):
        for hh in range(2):
            DMA(gq[hh * 64:(hh + 1) * 64, j:j + 1], sb_qg_d[j:j + 1, :].rearrange("o d -> d o"), 'gq', r=['gq'], w=['gq'], eng='act')
            DMA(gk[hh * 64:(hh + 1) * 64, j:j + 1], sb_kg_d[j:j + 1, :].rearrange("o d -> d o"), 'gk', r=['gk'], w=['gk'], eng='act')
    P.op('dve', lambda e: e.tensor_scalar(gq[:], gq[:], 0.125, None, ALU.mult), reads=['gq'], writes=['gq'])

    P.const_keys = CONSTS

    rot = {'all': [0, list(range(8))], 'A': [0, [0, 1, 2, 3]], 'B': [0, [4, 5]], 'C': [0, [6, 7]]}

    def bank(pool='all'):
        st = rot[pool]
        b = st[1][st[0] % len(st[1])]
        st[0] += 1
        return b

    wctr = [0, 0]

    def wissue(pieces, KC, N, ceng):
        si = wctr[0] % NS
        bi = wctr[1] % NB
        wctr[0] += 1
        wctr[1] += 1
        assert KC * N <= WCAP
        sv = wst[si][:, 0:KC * N].rearrange("p (c n) -> p c n", n=N)
        keys = []
        for pi, (ap, c0) in enumerate(pieces):
            n = ap.shape[2]
            k = ('wst', si, pi)
            keys.append(k)
            DMA(sv[:, :, c0:c0 + n], ap, k, w=[k])
        bv = wbf[bi][:, 0:KC * N]
        CP(ceng, bv, wst[si][:, 0:KC * N], keys, [('wbf', bi)], bar=False)
        return bv.rearrange("p (c n) -> p c n", n=N), ('wbf', bi)

    class WStream:
        def __init__(self, specs, engs, look=2):
            self.specs = specs
            self.engs = engs
            self.look = look
            self.i = 0
            self.loaded = []

        def next(self):
            while len(self.loaded) < min(len(self.specs), self.i + 1 + self.look):
                k = len(self.loaded)
                pieces, KC, N = self.specs[k]
                self.loaded.append(wissue(pieces, KC, N, self.engs[k % len(self.engs)]))
            r = self.loaded[self.i]
            self.i += 1
            return r

    def xk(c, g):
        return ('x', c, g)

    def hk(c, g):
        return ('h', c, g)

    def rmsnorm(gt, l):
        P.barrier()
        sq = aview(ARENA - 12 * 1024, [128, 8, 512], BF16)
        lnt = aview(ARENA - 4 * 1024, [128, 512], F32)
        rstd = aview(ARENA - 2 * 1024, [128, 512], F32)
        for g in range(4):
            ts = slice(g * 512, (g + 1) * 512)
            ACT(sq, xT[:, :, ts], AF.Square, [xk(c, g) for c in range(8)], ['n_sq'])
            b = bank()
            for c in range(8):
                MM(PS[b][:], ones_b[:], sq[:, c, :], c == 0, c == 7, ['n_sq'], [pk(b)], cr=['ones_b'])
            ACT(lnt, PS[b][:], AF.Ln, [pk(b)], ['n_ln'], scale=1.0 / D, bias=epscol[:])
            ACT(rstd, lnt, AF.Exp, ['n_ln'], ['n_rstd'], scale=-0.5)
            for c in range(8):
                STT(hT[:, c, ts], xT[:, c, ts], gt[:, l, c:c + 1], rstd, ALU.mult, ALU.mult,
                    [xk(c, g), 'n_rstd'], [hk(c, g)])
        P.barrier()

    def load_x(s):
        P.barrier()
        for tt in range(16):
            slot = tt % 2
            st = aview(slot * 4096, [128, D], F32)
            k = ('xst', slot)
            DMA(st, x_d[s, tt * 128:(tt + 1) * 128, :], k, w=[k], bar=True)
            for half in range(2):
                b = bank()
                for c in range(4):
                    cc = half * 4 + c
                    TR(PS[b][:, c * 128:(c + 1) * 128], st[:, cc * 128:(cc + 1) * 128], ident_f[:], [k], [pk(b)])
                CP('act' if half == 0 else 'dve', xT[:, half * 4:(half + 1) * 4, tt * 128:(tt + 1) * 128],
                   PS[b][:].rearrange("p (c t) -> p c t", t=128), [pk(b)], [xk(half * 4 + c, tt // 4) for c in range(4)])
        P.barrier()

    def store_x(s):
        P.barrier()
        outs = []
        for tt in range(16):
            slot = tt % 2
            st = aview(slot * 4096, [128, D], F32)
            k = ('xst', slot)
            for half in range(2):
                b = bank()
                for c in range(4):
                    cc = half * 4 + c
                    TR(PS[b][:, c * 128:(c + 1) * 128], xT[:, cc, tt * 128:(tt + 1) * 128], ident_f[:], [xk(cc, tt // 4)], [pk(b)])
                CP('act' if half == 0 else 'dve', st[:, half * 512:(half + 1) * 512], PS[b][:], [pk(b)], [('xo', slot, half)])
            o = DMA(y_d[s, tt * 128:(tt + 1) * 128, :], st, ('yst', slot), r=[('xo', slot, 0), ('xo', slot, 1)], bar=True)
            outs.append(o)
        P.barrier()
        return outs


    def ffn(l):
        rmsnorm(gffn, l)
        act = aview(0, [128, 11, S], BF16)
        sg = aview(44 * 1024, [128, 4, 512], BF16)
        wgu = ffn_wgu_d[l].rearrange("(c p) n -> p c n", p=128)
        wd = ffn_wd_d[l].rearrange("(c p) n -> p c n", p=128)
        specs = []
        for part in range(2):
            for jj in range(11):
                j = part * 11 + jj
                specs.append(([(wgu[:, :, j * 128:(j + 1) * 128], 0), (wgu[:, :, DFF + j * 128:DFF + (j + 1) * 128], 128)], 8, 256))
            for n in range(8):
                specs.append(([(wd[:, part * 11:(part + 1) * 11, n * 128:(n + 1) * 128], 0)], 11, 128))
        ws = WStream(specs, ['pool', 'dve'])
        for part in range(2):
            for jj in range(11):
                j = part * 11 + jj
                wv, wk = ws.next()
                for pas in range(2):
                    for kc in range(8):
                        for g in range(4):
                            b = pas * 4 + g
                            MM(PS[b][:], wv[:, kc, pas * 128:(pas + 1) * 128], hT[:, kc, g * 512:(g + 1) * 512],
                               kc == 0, kc == 7, [wk, hk(kc, g)], [pk(b)])
                    if pas == 0:
                        for g in range(4):
                            ACT(sg[:, g, :], PS[g][:], AF.Silu, [pk(g)], [('sg', g)])
                    else:
                        for g in range(4):
                            TT('dve', act[:, jj, g * 512:(g + 1) * 512], sg[:, g, :], PS[4 + g][:], ALU.mult,
                               [('sg', g), pk(4 + g)], [('act', jj, g)])
            for n in range(8):
                wv, wk = ws.next()
                b0 = (n % 2) * 4
                for kc in range(11):
                    for g in range(4):
                        MM(PS[b0 + g][:], wv[:, kc, :], act[:, kc, g * 512:(g + 1) * 512], kc == 0, kc == 10,
                           [wk, ('act', kc, g)], [pk(b0 + g)])
                for g in range(4):
                    TT('dve', xT[:, n, g * 512:(g + 1) * 512], PS[b0 + g][:], xT[:, n, g * 512:(g + 1) * 512], ALU.add,
                       [pk(b0 + g), xk(n, g)], [xk(n, g)])

    def out_specs(w_d):
        wo = w_d.rearrange("(c p) n -> p c n", p=128)
        return [([(wo[:, :, nb * 256:(nb + 1) * 256], 0)], 8, 256) for nb in range(4)]

    def out_proj(ws, src, srckey, groups):
        ng = len(groups)
        for nb in range(4):
            wv, wk = ws.next()
            for cc in range(2):
                n = nb * 2 + cc
                bs = [bank() for _ in groups]
                for kc in range(8):
                    for gi, g in enumerate(groups):
                        MM(PS[bs[gi]][:], wv[:, kc, cc * 128:(cc + 1) * 128], src[:, kc, g * 512:(g + 1) * 512],
                           kc == 0, kc == 7, [wk, srckey(kc, g)], [pk(bs[gi])])
                for gi, g in enumerate(groups):
                    TT('dve', xT[:, n, g * 512:(g + 1) * 512], PS[bs[gi]][:], xT[:, n, g * 512:(g + 1) * 512], ALU.add,
                       [pk(bs[gi]), xk(n, g)], [xk(n, g)])

    def gla(l, j):
        rmsnorm(gattn, l)
        K1 = 1024
        qT = aview(0, [128, 4, 512], BF16)
        kT = aview(4 * K1, [128, 4, 512], BF16)
        ktok = aview(8 * K1, [128, 4, 512], BF16)
        vv = aview(12 * K1, [128, 4, 1024], BF16)
        gsr = aview(20 * K1, [128, 4, 1024], BF16)
        glrT = aview(28 * K1, [32, 512], BF16)
        Eb = aview(29 * K1, [128, 512], F32)
        la = aview(31 * K1, [128, 512], F32)
        ebT = aview(33 * K1, [128, 512], F32)
        enbT = aview(35 * K1, [128, 512], F32)
        es = aview(37 * K1, [128, 512], F32)
        qf = aview(39 * K1, [128, 4, 128], BF16)
        qc1 = aview(40 * K1, [128, 4, 128], BF16)
        qc2 = aview(41 * K1, [128, 4, 128], BF16)
        ktl = aview(42 * K1, [128, 4, 128], BF16)
        ks = aview(43 * K1, [128, 512], BF16)
        scT = aview(44 * K1, [128, 4, 128], BF16)
        S32 = aview(45 * K1, [128, 4, 256], F32)
        Sa = aview(49 * K1, [128, 4, 256], BF16)
        Sb = aview(51 * K1, [128, 4, 256], BF16)
        yy = aview(53 * K1, [128, 1024], BF16)
        junk = aview(55 * K1, [128, 256], BF16)
        ss4 = aview(55 * K1 + 512, [128, 4], F32)
        t4 = aview(55 * K1 + 544, [128, 4], F32)
        rs4 = aview(55 * K1 + 576, [128, 4], F32)
        gfull = aview(56 * K1, [128, 1024], BF16)
        W2b = aview(58 * K1, [32, 512], BF16)
        silt = aview(62 * K1, [128, 2, 256], BF16)
        ebTs = [ebT, aview(59 * K1, [128, 512], F32)]
        scTs = [scT, aview(61 * K1, [128, 4, 128], BF16)]
        kss = [ks, aview(63 * K1, [128, 512], BF16)]
        qc1s = [qc1, aview(64 * K1, [128, 4, 128], BF16)]
        qc2s = [qc2, aview(65 * K1, [128, 4, 128], BF16)]

        win = gla_w_in_d[j].rearrange("(c p) n -> p c n", p=128)
        DMA(W2s[0:16, :], gla_w2_d[j], 'w2s', w=['w2s'])
        DMA(W2s[16:17, :], gla_b_d[j:j + 1, :], 'w2s', r=['w2s'], w=['w2s'])
        CP('pool', W2b[0:17, :], W2s[0:17, :], ['w2s'], ['w2b'])
        DMA(grow[:], gla_ng_d[j:j + 1, :], 'grow', w=['grow'])
        b = bank()
        MM(PS[b][:, 0:256], ones_f[:], grow[:], True, True, ['grow'], [pk(b)], cr=['ones_f'])
        for h in range(4):
            CP('dve', gfull[:, h * 256:(h + 1) * 256], PS[b][:, 0:256], [pk(b)], [('gfull', h)])
        MS('pool', glrT[:], 1.0, ['glrT'])
        for p_ in range(2):
            MS('pool', qc1s[p_], 0.0, [('qc1', p_)])
            MS('pool', qc2s[p_], 0.0, [('qc2', p_)])
        MS('dve', S32[:], 0.0, [('S32', h) for h in range(4)])
        MS('pool', Sb[:], 0.0, ['Sb'])
        scale = 128 ** -0.5
        specs = []
        for qt in range(4):
            for blk in range(4):
                specs.append(([(win[:, :, blk * 256:(blk + 1) * 256], 0)], 8, 256))
            specs.append(([(win[:, :, 3072:3088], 0)], 8, 16))
            for blk in range(10):
                c0 = 512 + blk * 256
                specs.append(([(win[:, :, c0:c0 + 256], 0)], 8, 256))
            specs += out_specs(gla_wo_d[j])
        ws = WStream(specs, ['dve', 'pool', 'act'])

        for qt in range(4):
            t0q = qt * 512
            for blk in range(4):
                wv, wk = ws.next()
                for cc in range(2):
                    n = (blk % 2) * 2 + cc
                    b = bank()
                    for kc in range(8):
                        MM(PS[b][:], wv[:, kc, cc * 128:(cc + 1) * 128], hT[:, kc, t0q:t0q + 512], kc == 0, kc == 7,
                           [wk, hk(kc, qt)], [pk(b)])
                    dst = qT if blk < 2 else kT
                    CP('act' if cc == 0 else 'dve', dst[:, n, :], PS[b][:], [pk(b)], [('qT' if blk < 2 else 'kT', n)])
            wv, wk = ws.next()
            b = bank()
            for kc in range(8):
                MM(PS[b][0:16, :], wv[:, kc, :], hT[:, kc, t0q:t0q + 512], kc == 0, kc == 7, [wk, hk(kc, qt)], [pk(b)])
            CP('act', glrT[0:16, :], PS[b][0:16, :], [pk(b)], ['glrT'])
            for blk in range(10):
                c0 = 512 + blk * 256
                wv, wk = ws.next()
                for tp in range(2):
                    b = bank()
                    for th in range(2):
                        tt = tp * 2 + th
                        for kc in range(8):
                            MM(PS[b][:, th * 256:(th + 1) * 256], hT[:, kc, t0q + tt * 128:t0q + (tt + 1) * 128], wv[:, kc, :],
                               kc == 0, kc == 7, [wk, hk(kc, qt)], [pk(b)])
                    pv = PS[b][:].rearrange("p (a n) -> p a n", n=256)
                    if blk < 2:
                        CP('dve', ktok[:, tp * 2:tp * 2 + 2, blk * 256:(blk + 1) * 256], pv, [pk(b)], [('ktok', tp * 2), ('ktok', tp * 2 + 1)])
                    elif blk < 6:
                        vb = blk - 2
                        CP('act', vv[:, tp * 2:tp * 2 + 2, vb * 256:(vb + 1) * 256], pv, [pk(b)], [('v', tp * 2, vb), ('v', tp * 2 + 1, vb)])
                    else:
                        rb = blk - 6
                        ACT(silt, pv, AF.Silu, [pk(b)], ['silt'])
                        for th in range(2):
                            tt = tp * 2 + th
                            TT('dve', gsr[:, tt, rb * 256:(rb + 1) * 256], silt[:, th, :], gfull[:, rb * 256:(rb + 1) * 256], ALU.mult,
                               ['silt', ('gfull', rb)], [('gsr', tt, rb)])
            def prepA_pe(tt):
                t0 = tt * 128
                MM(PS[0][:], glrT[0:17, t0:t0 + 128], W2b[0:17, :], True, True, ['glrT', 'w2b'], [pk(0)])

            def prepA_act(tt):
                ACT(Eb, PS[0][:], AF.Exp, [pk(0)], ['Eb'], scale=-1.0)
                ACT(la, Eb, AF.Ln, ['Eb'], ['la'], bias=onecol[:])

            def prepB_pe(tt):
                for h in range(4):
                    MM(PS[1][:, h * 128:(h + 1) * 128], la[:, h * 128:(h + 1) * 128], triS_f[:], True, True, ['la'], [pk(1)], cr=['triS_f'])
                MM(PS[0][:], triR_f[:], la, True, True, ['la'], [pk(0)], cr=['triR_f'])

            def prepB_act(tt):
                p = tt % 2
                ACT(ebTs[p], PS[1][:], AF.Exp, [pk(1)], [('ebT', p)])
                ACT(enbT, PS[1][:], AF.Exp, [pk(1)], ['enbT'], scale=-1.0)
                ACT(es, PS[0][:], AF.Exp, [pk(0)], ['es'])

            def prepB_dve(tt):
                t0 = tt * 128
                p = tt % 2
                STT(qf, qT[:, :, t0:t0 + 128], scale, ebTs[p].rearrange("p (h t) -> p h t", t=128), ALU.mult, ALU.mult,
                    [('qT', n) for n in range(4)] + [('ebT', p)], ['qf'])
                CP('pool', qc1s[p][:, :, 0:64], qf[:, :, 0:64], ['qf'], [('qc1', p)])
                CP('pool', qc2s[p][:, :, 64:128], qf[:, :, 64:128], ['qf'], [('qc2', p)])
                TT('dve', ktl, kT[:, :, t0:t0 + 128], enbT.rearrange("p (h t) -> p h t", t=128), ALU.mult,
                   [('kT', n) for n in range(4)] + ['enbT'], ['ktl'])
                TT('dve', kss[p], ktok[:, tt, :], es, ALU.mult, [('ktok', tt), 'es'], [('ks', p)])

            def prepC_pe(tt):
                for h in range(4):
                    MM(PS[3][:, h * 128:(h + 1) * 128], ktl[:, h, :], qf[:, h, :], True, True, ['ktl', 'qf'], [pk(3)])

            def prepC_dve(tt):
                p = tt % 2
                TT('dve', scTs[p], PS[3][:].rearrange("p (h t) -> p h t", t=128), maskG_b[:], ALU.mult, [pk(3)], [('scT', p)])

            def hv(h):
                return (4 + h // 2, slice((h % 2) * 256, (h % 2 + 1) * 256), 6 + h // 2,
                        slice((h % 2) * 256, (h % 2 + 1) * 256), slice(h * 256, (h + 1) * 256), slice(h * 128, (h + 1) * 128))

            def tile_step(tt, nx):
                p = tt % 2
                t0 = tt * 128
                ebT, ks, scT, qc1, qc2 = ebTs[p], kss[p], scTs[p], qc1s[p], qc2s[p]
                for h in range(4):
                    ob, osl, tb, tsl, vsl, ksl = hv(h)
                    MM(PS[ob][:, osl], scT[:, h, :], vv[:, tt, vsl], h % 2 == 0, False, [('scT', p), ('v', tt, h)], [pk(ob)])
                for h in range(4):
                    ob, osl, tb, tsl, vsl, ksl = hv(h)
                    MM(PS[tb][:, tsl], ks[0:64, ksl], vv[0:64, tt, vsl], True, True, [('ks', p), ('v', tt, h)], [pk(tb)])
                for h in range(4):
                    ob, osl, tb, tsl, vsl, ksl = hv(h)
                    MM(PS[ob][:, osl], qc1[:, h, :], Sb[:, h, :], False, False, [('qc1', p), 'Sb'], [pk(ob)])
                if nx:
                    prepA_pe(tt + 1)
                for h in range(4):
                    ob, osl, tb, tsl, vsl, ksl = hv(h)
                    STT(S32[:, h, :], S32[:, h, :], ebT[:, h * 128 + 63:h * 128 + 64], PS[tb][:, tsl], ALU.mult, ALU.add,
                        [('S32', h), ('ebT', p), pk(tb)], [('S32', h)])
                CP('act', Sa[:], S32[:], [('S32', h) for h in range(4)], ['Sa'])
                if nx:
                    prepA_act(tt + 1)
                for h in range(4):
                    ob, osl, tb, tsl, vsl, ksl = hv(h)
                    MM(PS[tb][:, tsl], ks[64:128, ksl], vv[64:128, tt, vsl], True, True, [('ks', p), ('v', tt, h)], [pk(tb)])
                for h in range(4):
                    ob, osl, tb, tsl, vsl, ksl = hv(h)
                    MM(PS[ob][:, osl], qc2[:, h, :], Sa[:, h, :], False, h % 2 == 1, [('qc2', p), 'Sa'], [pk(ob)])
                if nx:
                    prepB_pe(tt + 1)
                for h in range(4):
                    ob, osl, tb, tsl, vsl, ksl = hv(h)
                    STT(S32[:, h, :], S32[:, h, :], ebT[:, h * 128 + 127:h * 128 + 128], PS[tb][:, tsl], ALU.mult, ALU.add,
                        [('S32', h), ('ebT', p), pk(tb)], [('S32', h)])
                CP('act', Sb[:], S32[:], [('S32', h) for h in range(4)], ['Sb'])
                for h in range(4):
                    ob, osl, tb, tsl, vsl, ksl = hv(h)
                    ACT(junk, PS[ob][:, osl], AF.Square, [pk(ob)], ['junk', ('ss4', h)], accum_out=ss4[:, h:h + 1])
                ACT(t4, ss4, AF.Ln, [('ss4', h) for h in range(4)], ['t4'], scale=1.0 / 256.0, bias=epscol[:])
                ACT(rs4, t4, AF.Exp, ['t4'], ['rs4'], scale=-0.5)
                if nx:
                    prepB_act(tt + 1)
                for h in range(4):
                    ob, osl, tb, tsl, vsl, ksl = hv(h)
                    STT(yy[:, h * 256:(h + 1) * 256], PS[ob][:, osl], rs4[:, h:h + 1], gsr[:, tt, h * 256:(h + 1) * 256], ALU.mult, ALU.mult,
                        [pk(ob), 'rs4', ('gsr', tt, h)], [('yy', h)])
                ybank = PS[2][:].bitcast(BF16)
                for c in range(8):
                    TR(ybank[:, c * 128:(c + 1) * 128], yy[:, c * 128:(c + 1) * 128], ident_b[:], [('yy', c // 2)], [pk(2)])
                CP('act', hT[:, :, t0q + t0:t0q + t0 + 128], ybank.rearrange("p (c t) -> p c t", t=128), [pk(2)],
                   [hk(c, qt) for c in range(8)])
                if nx:
                    prepB_dve(tt + 1)
                    prepC_pe(tt + 1)
                    prepC_dve(tt + 1)

            prepA_pe(0)
            prepA_act(0)
            prepB_pe(0)
            prepB_act(0)
            prepB_dve(0)
            prepC_pe(0)
            prepC_dve(0)
            for tt in range(4):
                tile_step(tt, tt + 1 < 4)
            out_proj(ws, hT, hk, [qt])

    def sb(l, j):
        rmsnorm(gattn, l)
        K1 = 1024
        oT = aview(0, [128, 8, S], BF16)
        qTcs = [aview(32 * K1, [128, S], BF16), aview(66 * K1, [128, S], BF16)]
        kTcs = [aview(36 * K1, [128, S], BF16), aview(70 * K1, [128, S], BF16)]
        vpad = aview(40 * K1, [128, 16, 2, 128], BF16)
        Ef = [aview(48 * K1 + i * 2048, [128, 512], F32) for i in range(2)]
        spb = [aview(52 * K1 + i * 1024, [128, 512], BF16) for i in range(2)] + [aview(64 * K1, [128, 512], BF16)]
        wTb = [aview(54 * K1 + i * 1024, [128, 512], BF16) for i in range(2)]
        acc32 = aview(56 * K1, [128, 512], F32)
        accb = [aview(58 * K1 + i * 1024, [128, 512], BF16) for i in range(2)]
        sqb = [aview(60 * K1 + i * 1024, [128, 512], BF16) for i in range(2)]
        lnt = aview(62 * K1, [128, 512], F32)
        win = sb_w_in_d[j].rearrange("(c p) n -> p c n", p=128)
        MS('pool', vpad[:], 0.0, [('vpad', tq_, h_) for tq_ in range(4) for h_ in range(2)])
        nrm = [0]
        specs = []
        for c in range(8):
            specs.append(([(win[:, :, c * 128:(c + 1) * 128], 0), (win[:, :, D + c * 128:D + (c + 1) * 128], 128)], 8, 256))
            specs.append(([(win[:, :, 2 * D + c * 128:2 * D + (c + 1) * 128], 0)], 8, 128))
        specs += out_specs(sb_wo_d[j])
        ws = WStream(specs, ['pool', 'dve'], look=1)

        def qknorm(bs, dst, dkey, gvec):
            for g in range(4):
                i = nrm[0] % 2
                nrm[0] += 1
                ACT(sqb[i], PS[bs[g]][:], AF.Square, [pk(bs[g])], [('sqb', i)])
                cb = bank('C')
                MM(PS[cb][:], blk_b[:], sqb[i], True, True, [('sqb', i)], [pk(cb)], cr=['blk_b'])
                ACT(lnt, PS[cb][:], AF.Ln, [pk(cb)], ['lnt'], scale=1.0 / 64.0, bias=epscol[:])
                ACT(lnt, lnt, AF.Exp, ['lnt'], ['lnt'], scale=-0.5)
                STT(dst[:, g * 512:(g + 1) * 512], PS[bs[g]][:], gvec[:, j:j + 1], lnt, ALU.mult, ALU.mult,
                    [pk(bs[g]), 'lnt'], [dkey + (g,)])

        def make_units(wq, wqk_key, par):
            inj = {}
            for u in range(8):
                which, g = u // 4, u % 4
                base = 2 + u * 8
                dst = qTcs[par] if which == 0 else kTcs[par]
                dkey = ('qTc', par, g) if which == 0 else ('kTc', par, g)
                gvec = gq if which == 0 else gk
                i = u % 2

                def f_proj(which=which, g=g):
                    for kc in range(8):
                        MM(PS[6][:], wq[:, kc, which * 128:(which + 1) * 128], hT[:, kc, g * 512:(g + 1) * 512],
                           kc == 0, kc == 7, [wqk_key, hk(kc, g)], [pk(6)])

                def f_sq(i=i):
                    ACT(sqb[i], PS[6][:], AF.Square, [pk(6)], [('sqb', i)])

                def f_ss(i=i):
                    MM(PS[7][:], blk_b[:], sqb[i], True, True, [('sqb', i)], [pk(7)], cr=['blk_b'])

                def f_ln():
                    ACT(lnt, PS[7][:], AF.Ln, [pk(7)], ['lnt'], scale=1.0 / 64.0, bias=epscol[:])
                    ACT(lnt, lnt, AF.Exp, ['lnt'], ['lnt'], scale=-0.5)

                def f_stt(dst=dst, dkey=dkey, g=g, gvec=gvec):
                    STT(dst[:, g * 512:(g + 1) * 512], PS[6][:], gvec[:, j:j + 1], lnt, ALU.mult, ALU.mult,
                        [pk(6), 'lnt'], [dkey])

                for off, f in enumerate([f_proj, f_sq, f_ss, f_ln, f_stt]):
                    inj.setdefault(base + off, []).append(f)
            return inj

        for c in range(8):
            par = c % 2
            qTc, kTc = qTcs[par], kTcs[par]

            def qkproj(which):
                bs = [0, 1, 2, 3]
                for kc in range(8):
                    for g in range(4):
                        MM(PS[bs[g]][:], wqk[:, kc, which * 128:(which + 1) * 128], hT[:, kc, g * 512:(g + 1) * 512],
                           kc == 0, kc == 7, [wqkk, hk(kc, g)], [pk(bs[g])])
                return bs

            def vproj(tqs):
                for tq in tqs:
                    b = 4 + tq % 2
                    for th in range(4):
                        tt = tq * 4 + th
                        for kc in range(8):
                            MM(PS[b][:, th * 128:(th + 1) * 128], hT[:, kc, tt * 128:(tt + 1) * 128], wvv[:, kc, :],
                               kc == 0, kc == 7, [wvk, hk(kc, tq)], [pk(b)])
                    pv = PS[b][:].rearrange("p (a n) -> p a n", n=128)
                    CP('dve', vpad[:, tq * 4:tq * 4 + 4, 0, 0:64], pv[:, :, 0:64], [pk(b)], [('vpad', tq, 0)])
                    CP('dve', vpad[:, tq * 4:tq * 4 + 4, 1, 64:128], pv[:, :, 64:128], [pk(b)], [('vpad', tq, 1)])

            if c == 0:
                wqk, wqkk = ws.next()
                bs = qkproj(0)
                wvv, wvk = ws.next()
                vproj([0, 1])
                qknorm(bs, qTc, ('qTc', par), gq)
                bs = qkproj(1)
                vproj([2, 3])
                qknorm(bs, kTc, ('kTc', par), gk)
            else:
                wvv, wvk = ws.next()
                vproj([0, 1, 2, 3])
            inj = {}
            if c < 7:
                wqn, wqnk = ws.next()
                inj = make_units(wqn, wqnk, 1 - par)
            tiles = []
            for qg in range(4):
                for h in range(2):
                    kbs = list(reversed(range(4 * qg + 4)))
                    for ti, kb in enumerate(kbs):
                        tiles.append(dict(qg=qg, h=h, kb=kb, first=(ti == 0), last=(ti == len(kbs) - 1),
                                          firstq=(h == 0 and ti == 0), lastq=(h == 1 and ti == len(kbs) - 1)))
            nt = len(tiles)
            obank = {}

            def stageA(i):
                t = tiles[i]
                qg, h, kb = t['qg'], t['h'], t['kb']
                di = kb - 4 * qg
                q0 = max(di, 0) * 128
                nq = 512 - q0
                t['q0'], t['nq'], t['di'] = q0, nq, di
                hp = slice(h * 64, (h + 1) * 64)
                zb = bank('A')
                t['zb'] = zb
                e = i % 2
                MM(PS[zb][:, 0:nq], kTc[hp, kb * 128:(kb + 1) * 128], qTc[hp, qg * 512 + q0:(qg + 1) * 512], True, True,
                   [('kTc', par, kb // 4), ('qTc', par, qg)], [pk(zb)])
                ACT(Ef[e][:, 0:nq], PS[zb][:, 0:nq], AF.Exp, [pk(zb)], [('Ef', e)])

            def stageA2(i):
                t = tiles[i]
                nq, di = t['nq'], t['di']
                e = i % 2
                p3 = i % 3
                ACT(spb[p3][:, 0:nq], Ef[e][:, 0:nq], AF.Ln, [('Ef', e)], [('spb', p3)], bias=onecol[:])
                if di >= 0:
                    TT('pool', spb[p3][:, 0:128], spb[p3][:, 0:128], mask01_b[:], ALU.mult, [('spb', p3)], [('spb', p3)])

            def stageB(i):
                t = tiles[i]
                qg, h, kb, q0, nq, di, zb = t['qg'], t['h'], t['kb'], t['q0'], t['nq'], t['di'], t['zb']
                e = i % 2
                p3 = i % 3
                last_is_suffix = t['first'] and di < 0
                MM(PS[zb][:, 0:nq], negU_b[:], spb[p3][:, 0:nq], False, last_is_suffix, [('spb', p3)], [pk(zb)], cr=['negU_b'])
                if not t['first']:
                    MM(PS[zb][:, 0:nq], negO_b[:], accb[(i + 1) % 2][:, q0:512], False, di < 0, [('accb', (i + 1) % 2)], [pk(zb)], cr=['negO_b'])
                if di >= 0:
                    MM(PS[zb][:, 0:128], ident_b[:], negbig_b[:], False, True, [], [pk(zb)], cr=['ident_b', 'negbig_b'])
                ACT(wTb[e][:, 0:nq], PS[zb][:, 0:nq], AF.Exp, [pk(zb)], [('wTb', e)])
                if t['first']:
                    MS('dve', acc32, 0.0, ['acc32'])
                if not t['last']:
                    TT('dve', acc32[:, q0:512], acc32[:, q0:512], spb[p3][:, 0:nq], ALU.add, ['acc32', ('spb', p3)], ['acc32'])
                    CP('dve', accb[i % 2], acc32, ['acc32'], [('accb', i % 2)])

            def stageC(i):
                t = tiles[i]
                qg, h, kb, q0, nq = t['qg'], t['h'], t['kb'], t['q0'], t['nq']
                e = i % 2
                if t['firstq']:
                    ob = bank('B')
                    obank[qg] = ob
                    MM(PS[ob][:], zeros_b[:, 0:128], zeros_b[:], True, False, [], [pk(ob)], cr=['zeros_b'])
                ob = obank[qg]
                MM(PS[ob][:, q0:512], vpad[:, kb, h, :], wTb[e][:, 0:nq], False, t['lastq'], [('vpad', kb // 4, h), ('wTb', e)], [pk(ob)])
                if t['lastq']:
                    CP('dve', oT[:, c, qg * 512:(qg + 1) * 512], PS[ob][:], [pk(ob)], [('oT', c, qg)])

            for step in range(nt + 3):
                for f in inj.get(step, ()):
                    f()
                if step < nt:
                    stageA(step)
                if 0 <= step - 2 < nt:
                    stageB(step - 2)
                if step < nt:
                    stageA2(step)
                if 0 <= step - 3 < nt:
                    stageC(step - 3)
        out_proj(ws, oT, lambda kc, g: ('oT', kc, g), [0, 1, 2, 3])

    finals = []
    for s in range(nseq):
        load_x(s)
        for l in layers:
            if l % 2 == 0:
                gla(l, l // 2)
            else:
                sb(l, l // 2)
            ffn(l)
        finals += store_x(s)
    P.emit(final_wait_ops=finals)
    return nc, P


_CACHE = {}


def kernel(**inputs):
    x = np.ascontiguousarray(inputs['x'], dtype=np.float32)
    B = x.shape[0]
    nseq = B // NCORES
    key = (nseq,)
    if key not in _CACHE:
        _CACHE[key] = build(nseq, list(range(DEPTH)))[0]
    nc = _CACHE[key]
    names = ['attn_norm_g', 'ffn_norm_g', 'gla_w_in', 'gla_w_gate2', 'gla_b_gate', 'gla_norm_g', 'gla_w_out',
             'sb_w_in', 'sb_q_norm_g', 'sb_k_norm_g', 'sb_w_out', 'ffn_w_gate_up', 'ffn_w_down']
    shared = {n: np.ascontiguousarray(inputs[n], dtype=np.float32) for n in names}
    in_maps = []
    for c in range(NCORES):
        m = dict(shared)
        m['x'] = x[c * nseq:(c + 1) * nseq]
        in_maps.append(m)
    res = run_bass_kernel_spmd(nc, in_maps, core_ids=list(range(NCORES)))
    return np.concatenate([r['y'] for r in res.results], axis=0).astype(np.float32)
```

```python
import contextlib
import numpy as np
import concourse.bass as bass
import concourse.mybir as mybir
from concourse.bass_utils import run_bass_kernel_spmd

F32 = mybir.dt.float32
BF16 = mybir.dt.bfloat16
AF = mybir.ActivationFunctionType
ALU = mybir.AluOpType

D = 1024
S = 2048
DFF = 2816
KD = 512
GIN = 3088
DEPTH = 4
NCORES = 8
EPS = 1e-6


class Prog:
    def __init__(self, nc):
        self.nc = nc
        self.ops = []
        self.lw = {}
        self.rd = {}
        self.last_bar = {}
        self.bar_dmas = []
        self.bar_deps = set()
        self.bar_pending = set()
        self.const_keys = []
        self.const_done = set()

    def barrier(self):
        self.bar_deps = set(self.last_bar.values()) | set(self.bar_dmas)
        self.bar_dmas = []
        self.bar_pending = {'pe', 'act', 'dve', 'pool', 'sp'}

    def op(self, eng, fn, reads=(), writes=(), creads=(), dma=None, bar=None):
        if bar is None:
            bar = dma is None
        raw = set()
        oth = set()
        lw = self.lw
        for k in reads:
            w = lw.get(k)
            if w is not None:
                raw.add(w)
        for k in creads:
            w = lw.get(k)
            if w is not None:
                raw.add(w)
        for k in writes:
            w = lw.get(k)
            if w is not None:
                oth.add(w)
            r = self.rd.get(k)
            if r:
                oth.update(r.values())
        i = len(self.ops)
        deps = set()
        ops = self.ops
        for d in raw:
            de = ops[d]
            if de[3] is None and dma is None and de[0] == eng and eng == 'pe':
                continue
            deps.add(d)
        for d in oth:
            de = ops[d]
            if de[3] is None and dma is None and de[0] == eng and eng == 'pe':
                continue
            deps.add(d)
        if dma is None and self.const_keys and eng not in self.const_done:
            self.const_done.add(eng)
            for k in self.const_keys:
                w = lw.get(k)
                if w is not None and not (ops[w][3] is None and ops[w][0] == eng and eng == 'pe'):
                    deps.add(w)
        if bar and eng in self.bar_pending:
            self.bar_pending.discard(eng)
            for d in self.bar_deps:
                de = ops[d]
                if de[3] is None and dma is None and de[0] == eng and eng == 'pe':
                    continue
                deps.add(d)
        ops.append((eng, fn, deps, dma))
        for k in reads:
            self.rd.setdefault(k, {})[(eng, i if dma is not None else -1)] = i
        for k in writes:
            lw[k] = i
            self.rd[k] = {}
        if bar:
            if dma is None:
                self.last_bar[eng] = i
            else:
                self.bar_dmas.append(i)
        return i

    def emit(self, final_wait_ops=()):
        nc = self.nc
        ops = self.ops
        n = len(ops)
        needed = [False] * n
        for (eng, fn, deps, dma) in ops:
            for d in deps:
                needed[d] = True
        for d in final_wait_ops:
            needed[d] = True
        stack = contextlib.ExitStack()
        engsem = {}
        for e in ('pe', 'act', 'dve', 'pool', 'sp'):
            engsem[e] = stack.enter_context(nc.semaphore('s_' + e))
        dmasem = {}
        for (eng, fn, deps, dma) in ops:
            if dma is not None and dma not in dmasem:
                dmasem[dma] = stack.enter_context(nc.semaphore('d_%d' % len(dmasem)))
        tok = [None] * n
        cnt = {}
        for i, (eng, fn, deps, dma) in enumerate(ops):
            if dma is not None:
                cnt[dma] = cnt.get(dma, 0) + 16
                tok[i] = (dmasem[dma], cnt[dma], dma)
            elif needed[i]:
                cnt[eng] = cnt.get(eng, 0) + 1
                tok[i] = (engsem[eng], cnt[eng], eng)
        self.maxcnt = dict(cnt)
        per = {e: [] for e in ('pe', 'act', 'dve', 'pool', 'sp')}
        for i, o in enumerate(ops):
            per[o[0]].append(i)
        fw = list(final_wait_ops)

        def run(ename, e):
            waited = {}
            for i in per[ename]:
                eng, fn, deps, dma = ops[i]
                for d in sorted(deps):
                    s, v, key = tok[d]
                    if waited.get(key, 0) < v:
                        e.wait_ge(s, v)
                        waited[key] = v
                ins = fn(e)
                if dma is not None:
                    ins.then_inc(tok[i][0], 16)
                elif needed[i]:
                    ins.then_inc(tok[i][0], 1)
            if ename == 'sp':
                for d in fw:
                    s, v, key = tok[d]
                    if waited.get(key, 0) < v:
                        e.wait_ge(s, v)
                        waited[key] = v

        with stack:
            with nc.Block() as block:
                @block.tensor
                def _(e):
                    run('pe', e)

                @block.scalar
                def _(e):
                    run('act', e)

                @block.vector
                def _(e):
                    run('dve', e)

                @block.gpsimd
                def _(e):
                    run('pool', e)

                @block.sync
                def _(e):
                    run('sp', e)


ARENA = 66 * 1024
NS = 3
NB = 3
WCAP = 2048


def build(nseq, layers):
    nc = bass.Bass("TRN2", target_bir_lowering=False)
    P = Prog(nc)

    def din(name, shape):
        return nc.dram_tensor(name, list(shape), F32, kind="ExternalInput").ap()

    x_d = din("x", [nseq, S, D])
    attn_g_d = din("attn_norm_g", [DEPTH, D])
    ffn_g_d = din("ffn_norm_g", [DEPTH, D])
    gla_w_in_d = din("gla_w_in", [2, D, GIN])
    gla_w2_d = din("gla_w_gate2", [2, 16, KD])
    gla_b_d = din("gla_b_gate", [2, KD])
    gla_ng_d = din("gla_norm_g", [2, 256])
    gla_wo_d = din("gla_w_out", [2, D, D])
    sb_w_in_d = din("sb_w_in", [2, D, 3 * D])
    sb_qg_d = din("sb_q_norm_g", [2, 64])
    sb_kg_d = din("sb_k_norm_g", [2, 64])
    sb_wo_d = din("sb_w_out", [2, D, D])
    ffn_wgu_d = din("ffn_w_gate_up", [DEPTH, D, 2 * DFF])
    ffn_wd_d = din("ffn_w_down", [DEPTH, DFF, D])
    y_d = nc.dram_tensor("y", [nseq, S, D], F32, kind="ExternalOutput").ap()

    xT = nc.alloc_sbuf_tensor("xT", [128, 8, S], F32)
    hT = nc.alloc_sbuf_tensor("hT", [128, 8, S], BF16)
    wst = [nc.alloc_sbuf_tensor("wst%d" % i, [128, WCAP], F32) for i in range(NS)]
    wbf = [nc.alloc_sbuf_tensor("wbf%d" % i, [128, WCAP], BF16) for i in range(NB)]
    arena = nc.alloc_sbuf_tensor("arena", [128, ARENA // 2], BF16)
    ident_f = nc.alloc_sbuf_tensor("ident_f", [128, 128], F32)
    ident_b = nc.alloc_sbuf_tensor("ident_b", [128, 128], BF16)
    ones_b = nc.alloc_sbuf_tensor("ones_b", [128, 128], BF16)
    blk_b = nc.alloc_sbuf_tensor("blk_b", [128, 128], BF16)
    negU_b = nc.alloc_sbuf_tensor("negU_b", [128, 128], BF16)
    negO_b = nc.alloc_sbuf_tensor("negO_b", [128, 128], BF16)
    negbig_b = nc.alloc_sbuf_tensor("negbig_b", [128, 128], BF16)
    mask01_b = nc.alloc_sbuf_tensor("mask01_b", [128, 128], BF16)
    zeros_b = nc.alloc_sbuf_tensor("zeros_b", [128, 512], BF16)
    triS_f = nc.alloc_sbuf_tensor("triS_f", [128, 128], F32)
    triR_f = nc.alloc_sbuf_tensor("triR_f", [128, 128], F32)
    maskG_b = nc.alloc_sbuf_tensor("maskG_b", [128, 4, 128], BF16)
    onecol = nc.alloc_sbuf_tensor("onecol", [128, 1], F32)
    epscol = nc.alloc_sbuf_tensor("epscol", [128, 1], F32)
    gattn = nc.alloc_sbuf_tensor("gattn", [128, DEPTH, 8], F32)
    gffn = nc.alloc_sbuf_tensor("gffn", [128, DEPTH, 8], F32)
    gq = nc.alloc_sbuf_tensor("gq", [128, 2], F32)
    gk = nc.alloc_sbuf_tensor("gk", [128, 2], F32)
    ones_f = nc.alloc_sbuf_tensor("ones_f", [1, 128], F32)
    W2s = nc.alloc_sbuf_tensor("W2s", [32, 512], F32)
    grow = nc.alloc_sbuf_tensor("grow", [1, 256], F32)
    PS = [nc.alloc_psum_tensor("ps%d" % i, [128, 512], F32) for i in range(8)]

    def pk(i):
        return ('ps', i)

    def aview(off, shape, dt):
        nel = 1
        for s_ in shape[1:]:
            nel *= s_
        nb = nel * (4 if dt == F32 else 2)
        assert off % 4 == 0 and off + nb <= ARENA, (off, nb)
        ap = arena[:, off // 2:(off + nb) // 2]
        if dt == F32:
            ap = ap.bitcast(F32)
        if len(shape) == 3:
            ap = ap.rearrange("p (a b) -> p a b", b=shape[2])
        elif len(shape) == 4:
            ap = ap.rearrange("p (a b c) -> p a b c", b=shape[2], c=shape[3])
        if shape[0] != 128:
            ap = ap[0:shape[0]]
        return ap

    def MM(out, lhsT, rhs, start, stop, r, w, cr=()):
        P.op('pe', lambda e: e.matmul(out, lhsT, rhs, start=start, stop=stop, skip_group_check=True), reads=r, writes=w, creads=cr)

    def TR(out, in_, ident, r, w):
        P.op('pe', lambda e: e.transpose(out, in_, ident), reads=r, writes=w)

    def ACT(out, in_, func, r, w, scale=None, bias=None, accum_out=None):
        kw = {}
        if scale is not None:
            kw['scale'] = scale
        if bias is not None:
            kw['bias'] = bias
        if accum_out is not None:
            kw['accum_out'] = accum_out
        P.op('act', lambda e: e.activation(out, in_, func, **kw), reads=r, writes=w)

    def TT(eng, out, in0, in1, op, r, w):
        P.op(eng, lambda e: e.tensor_tensor(out, in0, in1, op), reads=r, writes=w)

    def STT(out, in0, scalar, in1, op0, op1, r, w):
        P.op('dve', lambda e: e.scalar_tensor_tensor(out, in0, scalar, in1, op0, op1), reads=r, writes=w)

    def CP(eng, out, in_, r, w, bar=None):
        if eng == 'act':
            P.op('act', lambda e: e.activation(out, in_, AF.Copy), reads=r, writes=w, bar=bar)
        else:
            P.op(eng, lambda e: e.tensor_copy(out, in_), reads=r, writes=w, bar=bar)

    def MS(eng, ap, val, w, r=()):
        P.op(eng, lambda e: e.memset(ap, val), reads=r, writes=w)

    def ASEL(ap, pattern, cmp, fill, base, cm, key):
        P.op('pool', lambda e: e.affine_select(ap, ap, pattern, cmp, fill, base=base, channel_multiplier=cm), reads=[key], writes=[key])

    def DMA(out, in_, key, r=(), w=(), eng='sp', bar=False, slow=False):
        if slow:
            return P.op(eng, lambda e: e.dma_start(out=out, in_=in_, allow_slow_non_contiguous=True), reads=r, writes=w, dma=key, bar=bar)
        return P.op(eng, lambda e: e.dma_start(out=out, in_=in_), reads=r, writes=w, dma=key, bar=bar)

    MS('pool', ident_f[:], 1.0, ['ident_f'])
    ASEL(ident_f[:], [[-1, 128]], ALU.is_equal, 0.0, 0, 1, 'ident_f')
    MS('pool', ident_b[:], 1.0, ['ident_b'])
    ASEL(ident_b[:], [[-1, 128]], ALU.is_equal, 0.0, 0, 1, 'ident_b')
    MS('pool', ones_b[:], 1.0, ['ones_b'])
    MS('pool', blk_b[:], 1.0, ['blk_b'])
    MS('pool', blk_b[0:64, 64:128], 0.0, ['blk_b'])
    MS('pool', blk_b[64:128, 0:64], 0.0, ['blk_b'])
    MS('pool', negU_b[:], -1.0, ['negU_b'])
    ASEL(negU_b[:], [[-1, 128]], ALU.is_ge, 0.0, 0, 1, 'negU_b')
    MS('pool', negO_b[:], -1.0, ['negO_b'])
    MS('pool', negbig_b[:], -30000.0, ['negbig_b'])
    ASEL(negbig_b[:], [[-1, 128]], ALU.is_ge, 0.0, 0, 1, 'negbig_b')
    MS('pool', mask01_b[:], 1.0, ['mask01_b'])
    ASEL(mask01_b[:], [[1, 128]], ALU.is_gt, 0.0, 0, -1, 'mask01_b')
    MS('pool', zeros_b[:], 0.0, ['zeros_b'])
    MS('pool', triS_f[:], -1.0 / 16.0, ['triS_f'])
    ASEL(triS_f[:], [[1, 128]], ALU.is_ge, 0.0, 0, -1, 'triS_f')
    MS('pool', triS_f[0:64, 64:128], 0.0, ['triS_f'])
    MS('pool', triR_f[:], -1.0 / 16.0, ['triR_f'])
    ASEL(triR_f[:], [[-1, 128]], ALU.is_gt, 0.0, 0, 1, 'triR_f')
    MS('pool', triR_f[64:128, 0:64], 0.0, ['triR_f'])
    MS('pool', maskG_b[:], 1.0, ['maskG_b'])
    ASEL(maskG_b[:], [[0, 4], [1, 128]], ALU.is_ge, 0.0, 0, -1, 'maskG_b')
    MS('pool', maskG_b[0:64, :, 64:128], 0.0, ['maskG_b'])
    MS('pool', onecol[:], 1.0, ['onecol'])
    MS('pool', epscol[:], EPS, ['epscol'])
    MS('pool', ones_f[:], 1.0, ['ones_f'])
    CONSTS = ['ident_f', 'ident_b', 'ones_b', 'blk_b', 'negU_b', 'negO_b', 'negbig_b', 'mask01_b', 'zeros_b',
              'triS_f', 'triR_f', 'maskG_b', 'onecol', 'epscol', 'ones_f', 'gattn', 'gffn', 'gq', 'gk']

    DMA(gattn[:], attn_g_d.rearrange("l (c p) -> p l c", p=128), 'gattn', w=['gattn'], eng='act', slow=True)
    DMA(gffn[:], ffn_g_d.rearrange("l (c p) -> p l c", p=128), 'gffn', w=['gffn'], eng='act', slow=True)
    for j in range(2):
        for hh in range(2):
            DMA(gq[hh * 64:(hh + 1) * 64, j:j + 1], sb_qg_d[j:j + 1, :].rearrange("o d -> d o"), 'gq', r=['gq'], w=['gq'], eng='act')
            DMA(gk[hh * 64:(hh + 1) * 64, j:j + 1], sb_kg_d[j:j + 1, :].rearrange("o d -> d o"), 'gk', r=['gk'], w=['gk'], eng='act')
    P.op('dve', lambda e: e.tensor_scalar(gq[:], gq[:], 0.125, None, ALU.mult), reads=['gq'], writes=['gq'])

    P.const_keys = CONSTS

    rot = {'all': [0, list(range(8))], 'A': [0, [0, 1, 2, 3]], 'B': [0, [4, 5]], 'C': [0, [6, 7]]}

    def bank(pool='all'):
        st = rot[pool]
        b = st[1][st[0] % len(st[1])]
        st[0] += 1
        return b

    wctr = [0, 0]

    def wissue(pieces, KC, N, ceng, nst):
        si = wctr[0] % nst
        bi = wctr[1] % NB
        wctr[0] += 1
        wctr[1] += 1
        assert KC * N <= WCAP
        sv = wst[si][:, 0:KC * N].rearrange("p (c n) -> p c n", n=N)
        keys = []
        for pi, (ap, c0) in enumerate(pieces):
            n = ap.shape[2]
            k = ('wst', si, pi)
            keys.append(k)
            DMA(sv[:, :, c0:c0 + n], ap, k, w=[k])
        bv = wbf[bi][:, 0:KC * N]
        CP(ceng, bv, wst[si][:, 0:KC * N], keys, [('wbf', bi)], bar=False)
        return bv.rearrange("p (c n) -> p c n", n=N), ('wbf', bi)

    class WStream:
        def __init__(self, specs, engs, look=2, nst=NS):
            self.nst = nst
            self.specs = specs
            self.engs = engs
            self.look = look
            self.i = 0
            self.loaded = []

        def next(self):
            while len(self.loaded) < min(len(self.specs), self.i + 1 + self.look):
                k = len(self.loaded)
                pieces, KC, N = self.specs[k]
                self.loaded.append(wissue(pieces, KC, N, self.engs[k % len(self.engs)], self.nst))
            r = self.loaded[self.i]
            self.i += 1
            return r

    def xk(c, g):
        return ('x', c, g)

    def hk(c, g):
        return ('h', c, g)

    def rmsnorm(gt, l):
        P.barrier()
        sq = aview(ARENA - 12 * 1024, [128, 8, 512], BF16)
        lnt = aview(ARENA - 4 * 1024, [128, 512], F32)
        rstd = aview(ARENA - 2 * 1024, [128, 512], F32)
        for g in range(4):
            ts = slice(g * 512, (g + 1) * 512)
            ACT(sq, xT[:, :, ts], AF.Square, [xk(c, g) for c in range(8)], ['n_sq'])
            b = bank()
            for c in range(8):
                MM(PS[b][:], ones_b[:], sq[:, c, :], c == 0, c == 7, ['n_sq'], [pk(b)], cr=['ones_b'])
            ACT(lnt, PS[b][:], AF.Ln, [pk(b)], ['n_ln'], scale=1.0 / D, bias=epscol[:])
            ACT(rstd, lnt, AF.Exp, ['n_ln'], ['n_rstd'], scale=-0.5)
            for c in range(8):
                STT(hT[:, c, ts], xT[:, c, ts], gt[:, l, c:c + 1], rstd, ALU.mult, ALU.mult,
                    [xk(c, g), 'n_rstd'], [hk(c, g)])
        P.barrier()

    def load_x(s):
        P.barrier()
        for tt in range(16):
            slot = tt % 2
            st = aview(slot * 4096, [128, D], F32)
            k = ('xst', slot)
            DMA(st, x_d[s, tt * 128:(tt + 1) * 128, :], k, w=[k], bar=True)
            for half in range(2):
                b = bank()
                for c in range(4):
                    cc = half * 4 + c
                    TR(PS[b][:, c * 128:(c + 1) * 128], st[:, cc * 128:(cc + 1) * 128], ident_f[:], [k], [pk(b)])
                CP('act' if half == 0 else 'dve', xT[:, half * 4:(half + 1) * 4, tt * 128:(tt + 1) * 128],
                   PS[b][:].rearrange("p (c t) -> p c t", t=128), [pk(b)], [xk(half * 4 + c, tt // 4) for c in range(4)])
        P.barrier()

    def store_x(s):
        P.barrier()
        outs = []
        for tt in range(16):
            slot = tt % 2
            st = aview(slot * 4096, [128, D], F32)
            k = ('xst', slot)
            for half in range(2):
                b = bank()
                for c in range(4):
                    cc = half * 4 + c
                    TR(PS[b][:, c * 128:(c + 1) * 128], xT[:, cc, tt * 128:(tt + 1) * 128], ident_f[:], [xk(cc, tt // 4)], [pk(b)])
                CP('act' if half == 0 else 'dve', st[:, half * 512:(half + 1) * 512], PS[b][:], [pk(b)], [('xo', slot, half)])
            o = DMA(y_d[s, tt * 128:(tt + 1) * 128, :], st, ('yst', slot), r=[('xo', slot, 0), ('xo', slot, 1)], bar=True)
            outs.append(o)
        P.barrier()
        return outs

    def swap_x(s_st, s_ld):
        P.barrier()
        outs = []
        for tt in range(16):
            slot = tt % 2
            if s_st is not None:
                st = aview(slot * 4096, [128, D], F32)
                for half in range(2):
                    b = bank()
                    for c in range(4):
                        cc = half * 4 + c
                        TR(PS[b][:, c * 128:(c + 1) * 128], xT[:, cc, tt * 128:(tt + 1) * 128], ident_f[:], [xk(cc, tt // 4)], [pk(b)])
                    CP('act' if half == 0 else 'dve', st[:, half * 512:(half + 1) * 512], PS[b][:], [pk(b)], [('xo', slot, half)])
                outs.append(DMA(y_d[s_st, tt * 128:(tt + 1) * 128, :], st, ('yst', slot), r=[('xo', slot, 0), ('xo', slot, 1)], bar=True))
            if s_ld is not None:
                st = aview(8192 + slot * 4096, [128, D], F32)
                k = ('xst', slot)
                DMA(st, x_d[s_ld, tt * 128:(tt + 1) * 128, :], k, w=[k], bar=True)
                for half in range(2):
                    b = bank()
                    for c in range(4):
                        cc = half * 4 + c
                        TR(PS[b][:, c * 128:(c + 1) * 128], st[:, cc * 128:(cc + 1) * 128], ident_f[:], [k], [pk(b)])
                    CP('dve' if half == 0 else 'act', xT[:, half * 4:(half + 1) * 4, tt * 128:(tt + 1) * 128],
                       PS[b][:].rearrange("p (c t) -> p c t", t=128), [pk(b)], [xk(half * 4 + c, tt // 4) for c in range(4)])
        P.barrier()
        return outs


    def ffn(l):
        rmsnorm(gffn, l)
        act = aview(0, [128, 11, S], BF16)
        sg = aview(44 * 1024, [128, 4, 512], BF16)
        wgu = ffn_wgu_d[l].rearrange("(c p) n -> p c n", p=128)
        wd = ffn_wd_d[l].rearrange("(c p) n -> p c n", p=128)
        specs = []
        for part in range(2):
            for jj in range(11):
                j = part * 11 + jj
                specs.append(([(wgu[:, :, j * 128:(j + 1) * 128], 0), (wgu[:, :, DFF + j * 128:DFF + (j + 1) * 128], 128)], 8, 256))
            for n in range(8):
                specs.append(([(wd[:, part * 11:(part + 1) * 11, n * 128:(n + 1) * 128], 0)], 11, 128))
        ws = WStream(specs, ['pool', 'dve'])
        for part in range(2):
            for jj in range(11):
                j = part * 11 + jj
                wv, wk = ws.next()
                for pas in range(2):
                    for kc in range(8):
                        for g in range(4):
                            b = pas * 4 + g
                            MM(PS[b][:], wv[:, kc, pas * 128:(pas + 1) * 128], hT[:, kc, g * 512:(g + 1) * 512],
                               kc == 0, kc == 7, [wk, hk(kc, g)], [pk(b)])
                    if pas == 0:
                        for g in range(4):
                            ACT(sg[:, g, :], PS[g][:], AF.Silu, [pk(g)], [('sg', g)])
                    else:
                        for g in range(4):
                            TT('dve', act[:, jj, g * 512:(g + 1) * 512], sg[:, g, :], PS[4 + g][:], ALU.mult,
                               [('sg', g), pk(4 + g)], [('act', jj, g)])
            for n in range(8):
                wv, wk = ws.next()
                b0 = (n % 2) * 4
                for kc in range(11):
                    for g in range(4):
                        MM(PS[b0 + g][:], wv[:, kc, :], act[:, kc, g * 512:(g + 1) * 512], kc == 0, kc == 10,
                           [wk, ('act', kc, g)], [pk(b0 + g)])
                for g in range(4):
                    TT('dve', xT[:, n, g * 512:(g + 1) * 512], PS[b0 + g][:], xT[:, n, g * 512:(g + 1) * 512], ALU.add,
                       [pk(b0 + g), xk(n, g)], [xk(n, g)])

    def out_specs(w_d):
        wo = w_d.rearrange("(c p) n -> p c n", p=128)
        return [([(wo[:, :, nb * 256:(nb + 1) * 256], 0)], 8, 256) for nb in range(4)]

    def out_proj(ws, src, srckey, groups):
        ng = len(groups)
        for nb in range(4):
            wv, wk = ws.next()
            for cc in range(2):
                n = nb * 2 + cc
                bs = [bank() for _ in groups]
                for kc in range(8):
                    for gi, g in enumerate(groups):
                        MM(PS[bs[gi]][:], wv[:, kc, cc * 128:(cc + 1) * 128], src[:, kc, g * 512:(g + 1) * 512],
                           kc == 0, kc == 7, [wk, srckey(kc, g)], [pk(bs[gi])])
                for gi, g in enumerate(groups):
                    TT('dve', xT[:, n, g * 512:(g + 1) * 512], PS[bs[gi]][:], xT[:, n, g * 512:(g + 1) * 512], ALU.add,
                       [pk(bs[gi]), xk(n, g)], [xk(n, g)])

    def gla(l, j):
        rmsnorm(gattn, l)
        K1 = 1024
        qT = aview(0, [128, 4, 512], BF16)
        kT = aview(4 * K1, [128, 4, 512], BF16)
        ktok = aview(8 * K1, [128, 4, 512], BF16)
        vv = aview(12 * K1, [128, 4, 1024], BF16)
        gsr = aview(20 * K1, [128, 4, 1024], BF16)
        glrT = aview(28 * K1, [32, 512], BF16)
        Eb = aview(29 * K1, [128, 512], F32)
        la = aview(31 * K1, [128, 512], F32)
        ebT = aview(33 * K1, [128, 512], F32)
        enbT = aview(35 * K1, [128, 512], F32)
        es = aview(37 * K1, [128, 512], F32)
        qf = aview(39 * K1, [128, 4, 128], BF16)
        qc1 = aview(40 * K1, [128, 4, 128], BF16)
        qc2 = aview(41 * K1, [128, 4, 128], BF16)
        ktl = aview(42 * K1, [128, 4, 128], BF16)
        ks = aview(43 * K1, [128, 512], BF16)
        scT = aview(44 * K1, [128, 4, 128], BF16)
        S32 = aview(45 * K1, [128, 4, 256], F32)
        Sa = aview(49 * K1, [128, 4, 256], BF16)
        Sb = aview(51 * K1, [128, 4, 256], BF16)
        yy = aview(53 * K1, [128, 1024], BF16)
        junk = aview(55 * K1, [128, 256], BF16)
        ss4 = aview(55 * K1 + 512, [128, 4], F32)
        t4 = aview(55 * K1 + 544, [128, 4], F32)
        rs4 = aview(55 * K1 + 576, [128, 4], F32)
        gfull = aview(56 * K1, [128, 1024], BF16)
        W2b = aview(58 * K1, [32, 512], BF16)
        silt = aview(62 * K1, [128, 2, 256], BF16)
        ebTs = [ebT, aview(59 * K1, [128, 512], F32)]
        scTs = [scT, aview(61 * K1, [128, 4, 128], BF16)]
        kss = [ks, aview(63 * K1, [128, 512], BF16)]
        qc1s = [qc1, aview(64 * K1, [128, 4, 128], BF16)]
        qc2s = [qc2, aview(65 * K1, [128, 4, 128], BF16)]

        win = gla_w_in_d[j].rearrange("(c p) n -> p c n", p=128)
        DMA(W2s[0:16, :], gla_w2_d[j], 'w2s', w=['w2s'])
        DMA(W2s[16:17, :], gla_b_d[j:j + 1, :], 'w2s', r=['w2s'], w=['w2s'])
        CP('pool', W2b[0:17, :], W2s[0:17, :], ['w2s'], ['w2b'])
        DMA(grow[:], gla_ng_d[j:j + 1, :], 'grow', w=['grow'])
        b = bank()
        MM(PS[b][:, 0:256], ones_f[:], grow[:], True, True, ['grow'], [pk(b)], cr=['ones_f'])
        for h in range(4):
            CP('dve', gfull[:, h * 256:(h + 1) * 256], PS[b][:, 0:256], [pk(b)], [('gfull', h)])
        MS('pool', glrT[:], 1.0, ['glrT'])
        for p_ in range(2):
            MS('pool', qc1s[p_], 0.0, [('qc1', p_)])
            MS('pool', qc2s[p_], 0.0, [('qc2', p_)])
        MS('dve', S32[:], 0.0, [('S32', h) for h in range(4)])
        MS('pool', Sb[:], 0.0, ['Sb'])
        scale = 128 ** -0.5
        specs = []
        for qt in range(4):
            for blk in range(4):
                specs.append(([(win[:, :, blk * 256:(blk + 1) * 256], 0)], 8, 256))
            specs.append(([(win[:, :, 3072:3088], 0)], 8, 16))
            for blk in range(10):
                c0 = 512 + blk * 256
                specs.append(([(win[:, :, c0:c0 + 256], 0)], 8, 256))
            specs += out_specs(gla_wo_d[j])
        ws = WStream(specs, ['dve', 'pool', 'act'])

        for qt in range(4):
            t0q = qt * 512
            for blk in range(4):
                wv, wk = ws.next()
                for cc in range(2):
                    n = (blk % 2) * 2 + cc
                    b = bank()
                    for kc in range(8):
                        MM(PS[b][:], wv[:, kc, cc * 128:(cc + 1) * 128], hT[:, kc, t0q:t0q + 512], kc == 0, kc == 7,
                           [wk, hk(kc, qt)], [pk(b)])
                    dst = qT if blk < 2 else kT
                    CP('act' if cc == 0 else 'dve', dst[:, n, :], PS[b][:], [pk(b)], [('qT' if blk < 2 else 'kT', n)])
            wv, wk = ws.next()
            b = bank()
            for kc in range(8):
                MM(PS[b][0:16, :], wv[:, kc, :], hT[:, kc, t0q:t0q + 512], kc == 0, kc == 7, [wk, hk(kc, qt)], [pk(b)])
            CP('act', glrT[0:16, :], PS[b][0:16, :], [pk(b)], ['glrT'])
            for blk in range(10):
                c0 = 512 + blk * 256
                wv, wk = ws.next()
                for tp in range(2):
                    b = bank()
                    for th in range(2):
                        tt = tp * 2 + th
                        for kc in range(8):
                            MM(PS[b][:, th * 256:(th + 1) * 256], hT[:, kc, t0q + tt * 128:t0q + (tt + 1) * 128], wv[:, kc, :],
                               kc == 0, kc == 7, [wk, hk(kc, qt)], [pk(b)])
                    pv = PS[b][:].rearrange("p (a n) -> p a n", n=256)
                    if blk < 2:
                        CP('dve', ktok[:, tp * 2:tp * 2 + 2, blk * 256:(blk + 1) * 256], pv, [pk(b)], [('ktok', tp * 2), ('ktok', tp * 2 + 1)])
                    elif blk < 6:
                        vb = blk - 2
                        CP('act', vv[:, tp * 2:tp * 2 + 2, vb * 256:(vb + 1) * 256], pv, [pk(b)], [('v', tp * 2, vb), ('v', tp * 2 + 1, vb)])
                    else:
                        rb = blk - 6
                        ACT(silt, pv, AF.Silu, [pk(b)], ['silt'])
                        for th in range(2):
                            tt = tp * 2 + th
                            TT('dve', gsr[:, tt, rb * 256:(rb + 1) * 256], silt[:, th, :], gfull[:, rb * 256:(rb + 1) * 256], ALU.mult,
                               ['silt', ('gfull', rb)], [('gsr', tt, rb)])
            def prepA_pe(tt):
                t0 = tt * 128
                MM(PS[0][:], glrT[0:17, t0:t0 + 128], W2b[0:17, :], True, True, ['glrT', 'w2b'], [pk(0)])

            def prepA_act(tt):
                ACT(Eb, PS[0][:], AF.Exp, [pk(0)], ['Eb'], scale=-1.0)
                ACT(la, Eb, AF.Ln, ['Eb'], ['la'], bias=onecol[:])

            def prepB_pe(tt):
                for h in range(4):
                    MM(PS[1][:, h * 128:(h + 1) * 128], la[:, h * 128:(h + 1) * 128], triS_f[:], True, True, ['la'], [pk(1)], cr=['triS_f'])
                MM(PS[0][:], triR_f[:], la, True, True, ['la'], [pk(0)], cr=['triR_f'])

            def prepB_act(tt):
                p = tt % 2
                ACT(ebTs[p], PS[1][:], AF.Exp, [pk(1)], [('ebT', p)])
                ACT(enbT, PS[1][:], AF.Exp, [pk(1)], ['enbT'], scale=-1.0)
                ACT(es, PS[0][:], AF.Exp, [pk(0)], ['es'])

            def prepB_dve(tt):
                t0 = tt * 128
                p = tt % 2
                STT(qf, qT[:, :, t0:t0 + 128], scale, ebTs[p].rearrange("p (h t) -> p h t", t=128), ALU.mult, ALU.mult,
                    [('qT', n) for n in range(4)] + [('ebT', p)], ['qf'])
                CP('pool', qc1s[p][:, :, 0:64], qf[:, :, 0:64], ['qf'], [('qc1', p)])
                CP('pool', qc2s[p][:, :, 64:128], qf[:, :, 64:128], ['qf'], [('qc2', p)])
                TT('dve', ktl, kT[:, :, t0:t0 + 128], enbT.rearrange("p (h t) -> p h t", t=128), ALU.mult,
                   [('kT', n) for n in range(4)] + ['enbT'], ['ktl'])
                TT('dve', kss[p], ktok[:, tt, :], es, ALU.mult, [('ktok', tt), 'es'], [('ks', p)])

            def prepC_pe(tt):
                for h in range(4):
                    MM(PS[3][:, h * 128:(h + 1) * 128], ktl[:, h, :], qf[:, h, :], True, True, ['ktl', 'qf'], [pk(3)])

            def prepC_dve(tt):
                p = tt % 2
                TT('dve', scTs[p], PS[3][:].rearrange("p (h t) -> p h t", t=128), maskG_b[:], ALU.mult, [pk(3)], [('scT', p)])

            def hv(h):
                return (4 + h // 2, slice((h % 2) * 256, (h % 2 + 1) * 256), 6 + h // 2,
                        slice((h % 2) * 256, (h % 2 + 1) * 256), slice(h * 256, (h + 1) * 256), slice(h * 128, (h + 1) * 128))

            def tile_step(tt, nx):
                p = tt % 2
                t0 = tt * 128
                ebT, ks, scT, qc1, qc2 = ebTs[p], kss[p], scTs[p], qc1s[p], qc2s[p]
                for h in range(4):
                    ob, osl, tb, tsl, vsl, ksl = hv(h)
                    MM(PS[ob][:, osl], scT[:, h, :], vv[:, tt, vsl], h % 2 == 0, False, [('scT', p), ('v', tt, h)], [pk(ob)])
                for h in range(4):
                    ob, osl, tb, tsl, vsl, ksl = hv(h)
                    MM(PS[tb][:, tsl], ks[0:64, ksl], vv[0:64, tt, vsl], True, True, [('ks', p), ('v', tt, h)], [pk(tb)])
                for h in range(4):
                    ob, osl, tb, tsl, vsl, ksl = hv(h)
                    MM(PS[ob][:, osl], qc1[:, h, :], Sb[:, h, :], False, False, [('qc1', p), 'Sb'], [pk(ob)])
                if nx:
                    prepA_pe(tt + 1)
                for h in range(4):
                    ob, osl, tb, tsl, vsl, ksl = hv(h)
                    STT(S32[:, h, :], S32[:, h, :], ebT[:, h * 128 + 63:h * 128 + 64], PS[tb][:, tsl], ALU.mult, ALU.add,
                        [('S32', h), ('ebT', p), pk(tb)], [('S32', h)])
                CP('act', Sa[:], S32[:], [('S32', h) for h in range(4)], ['Sa'])
                if nx:
                    prepA_act(tt + 1)
                for h in range(4):
                    ob, osl, tb, tsl, vsl, ksl = hv(h)
                    MM(PS[tb][:, tsl], ks[64:128, ksl], vv[64:128, tt, vsl], True, True, [('ks', p), ('v', tt, h)], [pk(tb)])
                for h in range(4):
                    ob, osl, tb, tsl, vsl, ksl = hv(h)
                    MM(PS[ob][:, osl], qc2[:, h, :], Sa[:, h, :], False, h % 2 == 1, [('qc2', p), 'Sa'], [pk(ob)])
                if nx:
                    prepB_pe(tt + 1)
                for h in range(4):
                    ob, osl, tb, tsl, vsl, ksl = hv(h)
                    STT(S32[:, h, :], S32[:, h, :], ebT[:, h * 128 + 127:h * 128 + 128], PS[tb][:, tsl], ALU.mult, ALU.add,
                        [('S32', h), ('ebT', p), pk(tb)], [('S32', h)])
                CP('act', Sb[:], S32[:], [('S32', h) for h in range(4)], ['Sb'])
                for h in range(4):
                    ob, osl, tb, tsl, vsl, ksl = hv(h)
                    ACT(junk, PS[ob][:, osl], AF.Square, [pk(ob)], ['junk', ('ss4', h)], accum_out=ss4[:, h:h + 1])
                ACT(t4, ss4, AF.Ln, [('ss4', h) for h in range(4)], ['t4'], scale=1.0 / 256.0, bias=epscol[:])
                ACT(rs4, t4, AF.Exp, ['t4'], ['rs4'], scale=-0.5)
                if nx:
                    prepB_act(tt + 1)
                for h in range(4):
                    ob, osl, tb, tsl, vsl, ksl = hv(h)
                    STT(yy[:, h * 256:(h + 1) * 256], PS[ob][:, osl], rs4[:, h:h + 1], gsr[:, tt, h * 256:(h + 1) * 256], ALU.mult, ALU.mult,
                        [pk(ob), 'rs4', ('gsr', tt, h)], [('yy', h)])
                ybank = PS[2][:].bitcast(BF16)
                for c in range(8):
                    TR(ybank[:, c * 128:(c + 1) * 128], yy[:, c * 128:(c + 1) * 128], ident_b[:], [('yy', c // 2)], [pk(2)])
                CP('act', hT[:, :, t0q + t0:t0q + t0 + 128], ybank.rearrange("p (c t) -> p c t", t=128), [pk(2)],
                   [hk(c, qt) for c in range(8)])
                if nx:
                    prepB_dve(tt + 1)
                    prepC_pe(tt + 1)
                    prepC_dve(tt + 1)

            prepA_pe(0)
            prepA_act(0)
            prepB_pe(0)
            prepB_act(0)
            prepB_dve(0)
            prepC_pe(0)
            prepC_dve(0)
            for tt in range(4):
                tile_step(tt, tt + 1 < 4)
            out_proj(ws, hT, hk, [qt])

    def sb(l, j):
        rmsnorm(gattn, l)
        K1 = 1024
        oT = aview(0, [128, 8, S], BF16)
        qTcs = [aview(32 * K1, [128, S], BF16), wst[2][:, 0:1024].bitcast(BF16)]
        kTcs = [aview(36 * K1, [128, S], BF16), wst[2][:, 1024:2048].bitcast(BF16)]
        XK = [('wst', 2, 0), ('wst', 2, 1)]
        vpad = aview(40 * K1, [128, 16, 2, 128], BF16)
        Ef = [aview(48 * K1 + i * 2048, [128, 512], F32) for i in range(2)]
        spb = [aview(52 * K1 + i * 1024, [128, 512], BF16) for i in range(2)] + [aview(64 * K1, [128, 512], BF16)]
        wTb = [aview(54 * K1 + i * 1024, [128, 512], BF16) for i in range(2)]
        acc32 = aview(56 * K1, [128, 512], F32)
        accb = [aview(58 * K1 + i * 1024, [128, 512], BF16) for i in range(2)]
        sqb = [aview(60 * K1 + i * 1024, [128, 512], BF16) for i in range(2)]
        lnt = aview(62 * K1, [128, 512], F32)
        win = sb_w_in_d[j].rearrange("(c p) n -> p c n", p=128)
        MS('pool', vpad[:], 0.0, [('vpad', tq_, h_) for tq_ in range(4) for h_ in range(2)])
        nrm = [0]
        specs = []
        for c in range(8):
            specs.append(([(win[:, :, c * 128:(c + 1) * 128], 0), (win[:, :, D + c * 128:D + (c + 1) * 128], 128)], 8, 256))
            specs.append(([(win[:, :, 2 * D + c * 128:2 * D + (c + 1) * 128], 0)], 8, 128))
        specs += out_specs(sb_wo_d[j])
        ws = WStream(specs, ['pool', 'dve'], look=1, nst=2)

        def qknorm(bs, dst, dkey, gvec):
            for g in range(4):
                i = nrm[0] % 2
                nrm[0] += 1
                ACT(sqb[i], PS[bs[g]][:], AF.Square, [pk(bs[g])], [('sqb', i)])
                cb = bank('C')
                MM(PS[cb][:], blk_b[:], sqb[i], True, True, [('sqb', i)], [pk(cb)], cr=['blk_b'])
                ACT(lnt, PS[cb][:], AF.Ln, [pk(cb)], ['lnt'], scale=1.0 / 64.0, bias=epscol[:])
                ACT(lnt, lnt, AF.Exp, ['lnt'], ['lnt'], scale=-0.5)
                STT(dst[:, g * 512:(g + 1) * 512], PS[bs[g]][:], gvec[:, j:j + 1], lnt, ALU.mult, ALU.mult,
                    [pk(bs[g]), 'lnt'], [dkey + (g,)])

        def make_units(wq, wqk_key, par):
            inj = {}
            for u in range(8):
                which, g = u // 4, u % 4
                base = 2 + u * 8
                dst = qTcs[par] if which == 0 else kTcs[par]
                dkey = ('qTc', par, g) if which == 0 else ('kTc', par, g)
                gvec = gq if which == 0 else gk
                i = u % 2

                def f_proj(which=which, g=g):
                    for kc in range(8):
                        MM(PS[6][:], wq[:, kc, which * 128:(which + 1) * 128], hT[:, kc, g * 512:(g + 1) * 512],
                           kc == 0, kc == 7, [wqk_key, hk(kc, g)], [pk(6)])

                def f_sq(i=i):
                    ACT(sqb[i], PS[6][:], AF.Square, [pk(6)], [('sqb', i)])

                def f_ss(i=i):
                    MM(PS[7][:], blk_b[:], sqb[i], True, True, [('sqb', i)], [pk(7)], cr=['blk_b'])

                def f_ln():
                    ACT(lnt, PS[7][:], AF.Ln, [pk(7)], ['lnt'], scale=1.0 / 64.0, bias=epscol[:])
                    ACT(lnt, lnt, AF.Exp, ['lnt'], ['lnt'], scale=-0.5)

                def f_stt(dst=dst, dkey=dkey, g=g, gvec=gvec):
                    STT(dst[:, g * 512:(g + 1) * 512], PS[6][:], gvec[:, j:j + 1], lnt, ALU.mult, ALU.mult,
                        [pk(6), 'lnt'], [dkey] + (XK if par == 1 else []))

                for off, f in enumerate([f_proj, f_sq, f_ss, f_ln, f_stt]):
                    inj.setdefault(base + off, []).append(f)
            return inj

        for c in range(8):
            par = c % 2
            qTc, kTc = qTcs[par], kTcs[par]

            def qkproj(which):
                bs = [0, 1, 2, 3]
                for kc in range(8):
                    for g in range(4):
                        MM(PS[bs[g]][:], wqk[:, kc, which * 128:(which + 1) * 128], hT[:, kc, g * 512:(g + 1) * 512],
                           kc == 0, kc == 7, [wqkk, hk(kc, g)], [pk(bs[g])])
                return bs

            def vproj(tqs):
                for tq in tqs:
                    b = 4 + tq % 2
                    for th in range(4):
                        tt = tq * 4 + th
                        for kc in range(8):
                            MM(PS[b][:, th * 128:(th + 1) * 128], hT[:, kc, tt * 128:(tt + 1) * 128], wvv[:, kc, :],
                               kc == 0, kc == 7, [wvk, hk(kc, tq)], [pk(b)])
                    pv = PS[b][:].rearrange("p (a n) -> p a n", n=128)
                    CP('dve', vpad[:, tq * 4:tq * 4 + 4, 0, 0:64], pv[:, :, 0:64], [pk(b)], [('vpad', tq, 0)])
                    CP('dve', vpad[:, tq * 4:tq * 4 + 4, 1, 64:128], pv[:, :, 64:128], [pk(b)], [('vpad', tq, 1)])

            if c == 0:
                wqk, wqkk = ws.next()
                bs = qkproj(0)
                wvv, wvk = ws.next()
                vproj([0, 1])
                qknorm(bs, qTc, ('qTc', par), gq)
                bs = qkproj(1)
                vproj([2, 3])
                qknorm(bs, kTc, ('kTc', par), gk)
            else:
                wvv, wvk = ws.next()
                vproj([0, 1, 2, 3])
            inj = {}
            if c < 7:
                wqn, wqnk = ws.next()
                inj = make_units(wqn, wqnk, 1 - par)
            tiles = []
            for qg in range(4):
                for h in range(2):
                    kbs = list(reversed(range(4 * qg + 4)))
                    for ti, kb in enumerate(kbs):
                        tiles.append(dict(qg=qg, h=h, kb=kb, first=(ti == 0), last=(ti == len(kbs) - 1),
                                          firstq=(h == 0 and ti == 0), lastq=(h == 1 and ti == len(kbs) - 1)))
            nt = len(tiles)
            obank = {}

            def stageA(i):
                t = tiles[i]
                qg, h, kb = t['qg'], t['h'], t['kb']
                di = kb - 4 * qg
                q0 = max(di, 0) * 128
                nq = 512 - q0
                t['q0'], t['nq'], t['di'] = q0, nq, di
                hp = slice(h * 64, (h + 1) * 64)
                zb = bank('A')
                t['zb'] = zb
                e = i % 2
                MM(PS[zb][:, 0:nq], kTc[hp, kb * 128:(kb + 1) * 128], qTc[hp, qg * 512 + q0:(qg + 1) * 512], True, True,
                   [('kTc', par, kb // 4), ('qTc', par, qg)] + (XK if par == 1 else []), [pk(zb)])
                ACT(Ef[e][:, 0:nq], PS[zb][:, 0:nq], AF.Exp, [pk(zb)], [('Ef', e)])

            def stageA2(i):
                t = tiles[i]
                nq, di = t['nq'], t['di']
                e = i % 2
                p3 = i % 3
                ACT(spb[p3][:, 0:nq], Ef[e][:, 0:nq], AF.Ln, [('Ef', e)], [('spb', p3)], bias=onecol[:])
                if di >= 0:
                    TT('pool', spb[p3][:, 0:128], spb[p3][:, 0:128], mask01_b[:], ALU.mult, [('spb', p3)], [('spb', p3)])

            def stageB(i):
                t = tiles[i]
                qg, h, kb, q0, nq, di, zb = t['qg'], t['h'], t['kb'], t['q0'], t['nq'], t['di'], t['zb']
                e = i % 2
                p3 = i % 3
                last_is_suffix = t['first'] and di < 0
                MM(PS[zb][:, 0:nq], negU_b[:], spb[p3][:, 0:nq], False, last_is_suffix, [('spb', p3)], [pk(zb)], cr=['negU_b'])
                if not t['first']:
                    MM(PS[zb][:, 0:nq], negO_b[:], accb[(i + 1) % 2][:, q0:512], False, di < 0, [('accb', (i + 1) % 2)], [pk(zb)], cr=['negO_b'])
                if di >= 0:
                    MM(PS[zb][:, 0:128], ident_b[:], negbig_b[:], False, True, [], [pk(zb)], cr=['ident_b', 'negbig_b'])
                ACT(wTb[e][:, 0:nq], PS[zb][:, 0:nq], AF.Exp, [pk(zb)], [('wTb', e)])
                if t['first']:
                    MS('dve', acc32, 0.0, ['acc32'])
                if not t['last']:
                    TT('dve', acc32[:, q0:512], acc32[:, q0:512], spb[p3][:, 0:nq], ALU.add, ['acc32', ('spb', p3)], ['acc32'])
                    CP('dve', accb[i % 2], acc32, ['acc32'], [('accb', i % 2)])

            def stageC(i):
                t = tiles[i]
                qg, h, kb, q0, nq = t['qg'], t['h'], t['kb'], t['q0'], t['nq']
                e = i % 2
                if t['firstq']:
                    ob = bank('B')
                    obank[qg] = ob
                    MM(PS[ob][:], zeros_b[:, 0:128], zeros_b[:], True, False, [], [pk(ob)], cr=['zeros_b'])
                ob = obank[qg]
                MM(PS[ob][:, q0:512], vpad[:, kb, h, :], wTb[e][:, 0:nq], False, t['lastq'], [('vpad', kb // 4, h), ('wTb', e)], [pk(ob)])
                if t['lastq']:
                    CP('dve', oT[:, c, qg * 512:(qg + 1) * 512], PS[ob][:], [pk(ob)], [('oT', c, qg)])

            for step in range(nt + 3):
                for f in inj.get(step, ()):
                    f()
                if step < nt:
                    stageA(step)
                if 0 <= step - 2 < nt:
                    stageB(step - 2)
                if step < nt:
                    stageA2(step)
                if 0 <= step - 3 < nt:
                    stageC(step - 3)
        out_proj(ws, oT, lambda kc, g: ('oT', kc, g), [0, 1, 2, 3])

    finals = []
    for s in range(nseq):
        if s == 0:
            swap_x(None, 0)
        for l in layers:
            if l % 2 == 0:
                gla(l, l // 2)
            else:
                sb(l, l // 2)
            ffn(l)
        finals += swap_x(s, s + 1 if s + 1 < nseq else None)
    P.emit(final_wait_ops=finals)
    return nc, P


_CACHE = {}


def kernel(**inputs):
    x = np.ascontiguousarray(inputs['x'], dtype=np.float32)
    B = x.shape[0]
    nseq = B // NCORES
    key = (nseq,)
    if key not in _CACHE:
        _CACHE[key] = build(nseq, list(range(DEPTH)))[0]
    nc = _CACHE[key]
    names = ['attn_norm_g', 'ffn_norm_g', 'gla_w_in', 'gla_w_gate2', 'gla_b_gate', 'gla_norm_g', 'gla_w_out',
             'sb_w_in', 'sb_q_norm_g', 'sb_k_norm_g', 'sb_w_out', 'ffn_w_gate_up', 'ffn_w_down']
    shared = {n: np.ascontiguousarray(inputs[n], dtype=np.float32) for n in names}
    in_maps = []
    for c in range(NCORES):
        m = dict(shared)
        m['x'] = x[c * nseq:(c + 1) * nseq]
        in_maps.append(m)
    res = run_bass_kernel_spmd(nc, in_maps, core_ids=list(range(NCORES)))
    return np.concatenate([r['y'] for r in res.results], axis=0).astype(np.float32)
```

```python
import contextlib
import numpy as np
import concourse.bass as bass
import concourse.mybir as mybir
from concourse.bass_utils import run_bass_kernel_spmd

F32 = mybir.dt.float32
BF16 = mybir.dt.bfloat16
AF = mybir.ActivationFunctionType
ALU = mybir.AluOpType

D = 1024
S = 2048
DFF = 2816
KD = 512
GIN = 3088
DEPTH = 4
NCORES = 8
EPS = 1e-6


class Prog:
    def __init__(self, nc):
        self.nc = nc
        self.ops = []
        self.lw = {}
        self.rd = {}
        self.last_bar = {}
        self.bar_dmas = []
        self.bar_deps = set()
        self.bar_pending = set()
        self.const_keys = []
        self.const_done = set()

    def barrier(self):
        self.bar_deps = set(self.last_bar.values()) | set(self.bar_dmas)
        self.bar_dmas = []
        self.bar_pending = {'pe', 'act', 'dve', 'pool', 'sp'}

    def op(self, eng, fn, reads=(), writes=(), creads=(), dma=None, bar=None):
        if bar is None:
            bar = dma is None
        raw = set()
        oth = set()
        lw = self.lw
        for k in reads:
            w = lw.get(k)
            if w is not None:
                raw.add(w)
        for k in creads:
            w = lw.get(k)
            if w is not None:
                raw.add(w)
        for k in writes:
            w = lw.get(k)
            if w is not None:
                oth.add(w)
            r = self.rd.get(k)
            if r:
                oth.update(r.values())
        i = len(self.ops)
        deps = set()
        ops = self.ops
        for d in raw:
            de = ops[d]
            if de[3] is None and dma is None and de[0] == eng and eng == 'pe':
                continue
            deps.add(d)
        for d in oth:
            de = ops[d]
            if de[3] is None and dma is None and de[0] == eng and eng == 'pe':
                continue
            deps.add(d)
        if dma is None and self.const_keys and eng not in self.const_done:
            self.const_done.add(eng)
            for k in self.const_keys:
                w = lw.get(k)
                if w is not None and not (ops[w][3] is None and ops[w][0] == eng and eng == 'pe'):
                    deps.add(w)
        if bar and eng in self.bar_pending:
            self.bar_pending.discard(eng)
            for d in self.bar_deps:
                de = ops[d]
                if de[3] is None and dma is None and de[0] == eng and eng == 'pe':
                    continue
                deps.add(d)
        ops.append((eng, fn, deps, dma))
        for k in reads:
            self.rd.setdefault(k, {})[(eng, i if dma is not None else -1)] = i
        for k in writes:
            lw[k] = i
            self.rd[k] = {}
        if bar:
            if dma is None:
                self.last_bar[eng] = i
            else:
                self.bar_dmas.append(i)
        return i

    def emit(self, final_wait_ops=()):
        nc = self.nc
        ops = self.ops
        n = len(ops)
        needed = [False] * n
        for (eng, fn, deps, dma) in ops:
            for d in deps:
                needed[d] = True
        for d in final_wait_ops:
            needed[d] = True
        stack = contextlib.ExitStack()
        engsem = {}
        for e in ('pe', 'act', 'dve', 'pool', 'sp'):
            engsem[e] = stack.enter_context(nc.semaphore('s_' + e))
        dmasem = {}
        for (eng, fn, deps, dma) in ops:
            if dma is not None and dma not in dmasem:
                dmasem[dma] = stack.enter_context(nc.semaphore('d_%d' % len(dmasem)))
        tok = [None] * n
        cnt = {}
        for i, (eng, fn, deps, dma) in enumerate(ops):
            if dma is not None:
                cnt[dma] = cnt.get(dma, 0) + 16
                tok[i] = (dmasem[dma], cnt[dma], dma)
            elif needed[i]:
                cnt[eng] = cnt.get(eng, 0) + 1
                tok[i] = (engsem[eng], cnt[eng], eng)
        self.maxcnt = dict(cnt)
        per = {e: [] for e in ('pe', 'act', 'dve', 'pool', 'sp')}
        for i, o in enumerate(ops):
            per[o[0]].append(i)
        fw = list(final_wait_ops)

        def run(ename, e):
            waited = {}
            for i in per[ename]:
                eng, fn, deps, dma = ops[i]
                for d in sorted(deps):
                    s, v, key = tok[d]
                    if waited.get(key, 0) < v:
                        e.wait_ge(s, v)
                        waited[key] = v
                ins = fn(e)
                if dma is not None:
                    ins.then_inc(tok[i][0], 16)
                elif needed[i]:
                    ins.then_inc(tok[i][0], 1)
            if ename == 'sp':
                for d in fw:
                    s, v, key = tok[d]
                    if waited.get(key, 0) < v:
                        e.wait_ge(s, v)
                        waited[key] = v

        with stack:
            with nc.Block() as block:
                @block.tensor
                def _(e):
                    run('pe', e)

                @block.scalar
                def _(e):
                    run('act', e)

                @block.vector
                def _(e):
                    run('dve', e)

                @block.gpsimd
                def _(e):
                    run('pool', e)

                @block.sync
                def _(e):
                    run('sp', e)


ARENA = 66 * 1024
NS = 3
NB = 3
WCAP = 2048


def build(nseq, layers):
    nc = bass.Bass("TRN2", target_bir_lowering=False)
    P = Prog(nc)

    def din(name, shape):
        return nc.dram_tensor(name, list(shape), F32, kind="ExternalInput").ap()

    x_d = din("x", [nseq, S, D])
    attn_g_d = din("attn_norm_g", [DEPTH, D])
    ffn_g_d = din("ffn_norm_g", [DEPTH, D])
    gla_w_in_d = din("gla_w_in", [2, D, GIN])
    gla_w2_d = din("gla_w_gate2", [2, 16, KD])
    gla_b_d = din("gla_b_gate", [2, KD])
    gla_ng_d = din("gla_norm_g", [2, 256])
    gla_wo_d = din("gla_w_out", [2, D, D])
    sb_w_in_d = din("sb_w_in", [2, D, 3 * D])
    sb_qg_d = din("sb_q_norm_g", [2, 64])
    sb_kg_d = din("sb_k_norm_g", [2, 64])
    sb_wo_d = din("sb_w_out", [2, D, D])
    ffn_wgu_d = din("ffn_w_gate_up", [DEPTH, D, 2 * DFF])
    ffn_wd_d = din("ffn_w_down", [DEPTH, DFF, D])
    y_d = nc.dram_tensor("y", [nseq, S, D], F32, kind="ExternalOutput").ap()

    xT = nc.alloc_sbuf_tensor("xT", [128, 8, S], F32)
    hT = nc.alloc_sbuf_tensor("hT", [128, 8, S], BF16)
    wst = [nc.alloc_sbuf_tensor("wst%d" % i, [128, WCAP], F32) for i in range(NS)]
    wbf = [nc.alloc_sbuf_tensor("wbf%d" % i, [128, WCAP], BF16) for i in range(NB)]
    arena = nc.alloc_sbuf_tensor("arena", [128, ARENA // 2], BF16)
    ident_f = nc.alloc_sbuf_tensor("ident_f", [128, 128], F32)
    ident_b = nc.alloc_sbuf_tensor("ident_b", [128, 128], BF16)
    ones_b = nc.alloc_sbuf_tensor("ones_b", [128, 128], BF16)
    blk_b = nc.alloc_sbuf_tensor("blk_b", [128, 128], BF16)
    negU_b = nc.alloc_sbuf_tensor("negU_b", [128, 128], BF16)
    negO_b = nc.alloc_sbuf_tensor("negO_b", [128, 128], BF16)
    negbig_b = nc.alloc_sbuf_tensor("negbig_b", [128, 128], BF16)
    mask01_b = nc.alloc_sbuf_tensor("mask01_b", [128, 128], BF16)
    zeros_b = nc.alloc_sbuf_tensor("zeros_b", [128, 512], BF16)
    triS_f = nc.alloc_sbuf_tensor("triS_f", [128, 128], F32)
    triR_f = nc.alloc_sbuf_tensor("triR_f", [128, 128], F32)
    maskG_b = nc.alloc_sbuf_tensor("maskG_b", [128, 4, 128], BF16)
    onecol = nc.alloc_sbuf_tensor("onecol", [128, 1], F32)
    epscol = nc.alloc_sbuf_tensor("epscol", [128, 1], F32)
    gattn = nc.alloc_sbuf_tensor("gattn", [128, DEPTH, 8], F32)
    gffn = nc.alloc_sbuf_tensor("gffn", [128, DEPTH, 8], F32)
    gq = nc.alloc_sbuf_tensor("gq", [128, 2], F32)
    gk = nc.alloc_sbuf_tensor("gk", [128, 2], F32)
    ones_f = nc.alloc_sbuf_tensor("ones_f", [1, 128], F32)
    W2s = nc.alloc_sbuf_tensor("W2s", [32, 512], F32)
    grow = nc.alloc_sbuf_tensor("grow", [1, 256], F32)
    PS = [nc.alloc_psum_tensor("ps%d" % i, [128, 512], F32) for i in range(8)]

    def pk(i):
        return ('ps', i)

    def aview(off, shape, dt):
        nel = 1
        for s_ in shape[1:]:
            nel *= s_
        nb = nel * (4 if dt == F32 else 2)
        assert off % 4 == 0 and off + nb <= ARENA, (off, nb)
        ap = arena[:, off // 2:(off + nb) // 2]
        if dt == F32:
            ap = ap.bitcast(F32)
        if len(shape) == 3:
            ap = ap.rearrange("p (a b) -> p a b", b=shape[2])
        elif len(shape) == 4:
            ap = ap.rearrange("p (a b c) -> p a b c", b=shape[2], c=shape[3])
        if shape[0] != 128:
            ap = ap[0:shape[0]]
        return ap

    def MM(out, lhsT, rhs, start, stop, r, w, cr=()):
        P.op('pe', lambda e: e.matmul(out, lhsT, rhs, start=start, stop=stop, skip_group_check=True), reads=r, writes=w, creads=cr)

    def TR(out, in_, ident, r, w):
        P.op('pe', lambda e: e.transpose(out, in_, ident), reads=r, writes=w)

    def ACT(out, in_, func, r, w, scale=None, bias=None, accum_out=None):
        kw = {}
        if scale is not None:
            kw['scale'] = scale
        if bias is not None:
            kw['bias'] = bias
        if accum_out is not None:
            kw['accum_out'] = accum_out
        P.op('act', lambda e: e.activation(out, in_, func, **kw), reads=r, writes=w)

    def TT(eng, out, in0, in1, op, r, w):
        P.op(eng, lambda e: e.tensor_tensor(out, in0, in1, op), reads=r, writes=w)

    def STT(out, in0, scalar, in1, op0, op1, r, w):
        P.op('dve', lambda e: e.scalar_tensor_tensor(out, in0, scalar, in1, op0, op1), reads=r, writes=w)

    def CP(eng, out, in_, r, w, bar=None):
        if eng == 'act':
            P.op('act', lambda e: e.activation(out, in_, AF.Copy), reads=r, writes=w, bar=bar)
        else:
            P.op(eng, lambda e: e.tensor_copy(out, in_), reads=r, writes=w, bar=bar)

    def MS(eng, ap, val, w, r=()):
        P.op(eng, lambda e: e.memset(ap, val), reads=r, writes=w)

    def ASEL(ap, pattern, cmp, fill, base, cm, key):
        P.op('pool', lambda e: e.affine_select(ap, ap, pattern, cmp, fill, base=base, channel_multiplier=cm), reads=[key], writes=[key])

    def DMA(out, in_, key, r=(), w=(), eng='sp', bar=False, slow=False):
        if slow:
            return P.op(eng, lambda e: e.dma_start(out=out, in_=in_, allow_slow_non_contiguous=True), reads=r, writes=w, dma=key, bar=bar)
        return P.op(eng, lambda e: e.dma_start(out=out, in_=in_), reads=r, writes=w, dma=key, bar=bar)

    MS('pool', ident_f[:], 1.0, ['ident_f'])
    ASEL(ident_f[:], [[-1, 128]], ALU.is_equal, 0.0, 0, 1, 'ident_f')
    MS('pool', ident_b[:], 1.0, ['ident_b'])
    ASEL(ident_b[:], [[-1, 128]], ALU.is_equal, 0.0, 0, 1, 'ident_b')
    MS('pool', ones_b[:], 1.0, ['ones_b'])
    MS('pool', blk_b[:], 1.0, ['blk_b'])
    MS('pool', blk_b[0:64, 64:128], 0.0, ['blk_b'])
    MS('pool', blk_b[64:128, 0:64], 0.0, ['blk_b'])
    MS('pool', negU_b[:], -1.0, ['negU_b'])
    ASEL(negU_b[:], [[-1, 128]], ALU.is_ge, 0.0, 0, 1, 'negU_b')
    MS('pool', negO_b[:], -1.0, ['negO_b'])
    MS('pool', negbig_b[:], -30000.0, ['negbig_b'])
    ASEL(negbig_b[:], [[-1, 128]], ALU.is_ge, 0.0, 0, 1, 'negbig_b')
    MS('pool', mask01_b[:], 1.0, ['mask01_b'])
    ASEL(mask01_b[:], [[1, 128]], ALU.is_gt, 0.0, 0, -1, 'mask01_b')
    MS('pool', zeros_b[:], 0.0, ['zeros_b'])
    MS('pool', triS_f[:], -1.0 / 16.0, ['triS_f'])
    ASEL(triS_f[:], [[1, 128]], ALU.is_ge, 0.0, 0, -1, 'triS_f')
    MS('pool', triS_f[0:64, 64:128], 0.0, ['triS_f'])
    MS('pool', triR_f[:], -1.0 / 16.0, ['triR_f'])
    ASEL(triR_f[:], [[-1, 128]], ALU.is_gt, 0.0, 0, 1, 'triR_f')
    MS('pool', triR_f[64:128, 0:64], 0.0, ['triR_f'])
    MS('pool', maskG_b[:], 1.0, ['maskG_b'])
    ASEL(maskG_b[:], [[0, 4], [1, 128]], ALU.is_ge, 0.0, 0, -1, 'maskG_b')
    MS('pool', maskG_b[0:64, :, 64:128], 0.0, ['maskG_b'])
    MS('pool', onecol[:], 1.0, ['onecol'])
    MS('pool', epscol[:], EPS, ['epscol'])
    MS('pool', ones_f[:], 1.0, ['ones_f'])
    CONSTS = ['ident_f', 'ident_b', 'ones_b', 'blk_b', 'negU_b', 'negO_b', 'negbig_b', 'mask01_b', 'zeros_b',
              'triS_f', 'triR_f', 'maskG_b', 'onecol', 'epscol', 'ones_f', 'gattn', 'gffn', 'gq', 'gk']

    DMA(gattn[:], attn_g_d.rearrange("l (c p) -> p l c", p=128), 'gattn', w=['gattn'], eng='act', slow=True)
    DMA(gffn[:], ffn_g_d.rearrange("l (c p) -> p l c", p=128), 'gffn', w=['gffn'], eng='act', slow=True)
    for j in range(2):
        for hh in range(2):
            DMA(gq[hh * 64:(hh + 1) * 64, j:j + 1], sb_qg_d[j:j + 1, :].rearrange("o d -> d o"), 'gq', r=['gq'], w=['gq'], eng='act')
            DMA(gk[hh * 64:(hh + 1) * 64, j:j + 1], sb_kg_d[j:j + 1, :].rearrange("o d -> d o"), 'gk', r=['gk'], w=['gk'], eng='act')
    P.op('dve', lambda e: e.tensor_scalar(gq[:], gq[:], 0.125, None, ALU.mult), reads=['gq'], writes=['gq'])

    P.const_keys = CONSTS

    rot = {'all': [0, list(range(8))], 'A': [0, [0, 1, 2, 3]], 'B': [0, [4, 5]], 'C': [0, [6, 7]]}

    def bank(pool='all'):
        st = rot[pool]
        b = st[1][st[0] % len(st[1])]
        st[0] += 1
        return b

    wctr = [0, 0]

    def wissue(pieces, KC, N, ceng, nst):
        si = wctr[0] % nst
        bi = wctr[1] % NB
        wctr[0] += 1
        wctr[1] += 1
        assert KC * N <= WCAP
        sv = wst[si][:, 0:KC * N].rearrange("p (c n) -> p c n", n=N)
        keys = []
        for pi, (ap, c0) in enumerate(pieces):
            n = ap.shape[2]
            k = ('wst', si, pi)
            keys.append(k)
            DMA(sv[:, :, c0:c0 + n], ap, k, w=[k])
        bv = wbf[bi][:, 0:KC * N]
        CP(ceng, bv, wst[si][:, 0:KC * N], keys, [('wbf', bi)], bar=False)
        return bv.rearrange("p (c n) -> p c n", n=N), ('wbf', bi)

    class WStream:
        def __init__(self, specs, engs, look=2, nst=NS):
            self.nst = nst
            self.specs = specs
            self.engs = engs
            self.look = look
            self.i = 0
            self.loaded = []

        def next(self):
            while len(self.loaded) < min(len(self.specs), self.i + 1 + self.look):
                k = len(self.loaded)
                pieces, KC, N = self.specs[k]
                self.loaded.append(wissue(pieces, KC, N, self.engs[k % len(self.engs)], self.nst))
            r = self.loaded[self.i]
            self.i += 1
            return r

    def xk(c, g):
        return ('x', c, g)

    def hk(c, g):
        return ('h', c, g)

    def rmsnorm(gt, l):
        P.barrier()
        sqs = [aview(ARENA - 24 * 1024 + i * 8192, [128, 8, 512], BF16) for i in range(2)]
        lnts = [aview(ARENA - 8 * 1024 + i * 2048, [128, 512], F32) for i in range(2)]
        rstds = [aview(ARENA - 4 * 1024 + i * 2048, [128, 512], F32) for i in range(2)]
        def sqr(g):
            ts = slice(g * 512, (g + 1) * 512)
            ACT(sqs[g % 2], xT[:, :, ts], AF.Square, [xk(c, g) for c in range(8)], [('n_sq', g % 2)])

        sqr(0)
        for g in range(4):
            i = g % 2
            sq, lnt, rstd = sqs[i], lnts[i], rstds[i]
            ts = slice(g * 512, (g + 1) * 512)
            b = bank()
            for c in range(8):
                MM(PS[b][:], ones_b[:], sq[:, c, :], c == 0, c == 7, [('n_sq', i)], [pk(b)], cr=['ones_b'])
            if g + 1 < 4:
                sqr(g + 1)
            ACT(lnt, PS[b][:], AF.Ln, [pk(b)], [('n_ln', i)], scale=1.0 / D, bias=epscol[:])
            ACT(rstd, lnt, AF.Exp, [('n_ln', i)], [('n_rstd', i)], scale=-0.5)
            for c in range(8):
                STT(hT[:, c, ts], xT[:, c, ts], gt[:, l, c:c + 1], rstd, ALU.mult, ALU.mult,
                    [xk(c, g), ('n_rstd', i)], [hk(c, g)])
        P.barrier()

    def load_x(s):
        P.barrier()
        for tt in range(16):
            slot = tt % 2
            st = aview(slot * 4096, [128, D], F32)
            k = ('xst', slot)
            DMA(st, x_d[s, tt * 128:(tt + 1) * 128, :], k, w=[k], bar=True)
            for half in range(2):
                b = bank()
                for c in range(4):
                    cc = half * 4 + c
                    TR(PS[b][:, c * 128:(c + 1) * 128], st[:, cc * 128:(cc + 1) * 128], ident_f[:], [k], [pk(b)])
                CP('act' if half == 0 else 'dve', xT[:, half * 4:(half + 1) * 4, tt * 128:(tt + 1) * 128],
                   PS[b][:].rearrange("p (c t) -> p c t", t=128), [pk(b)], [xk(half * 4 + c, tt // 4) for c in range(4)])
        P.barrier()

    def store_x(s):
        P.barrier()
        outs = []
        for tt in range(16):
            slot = tt % 2
            st = aview(slot * 4096, [128, D], F32)
            k = ('xst', slot)
            for half in range(2):
                b = bank()
                for c in range(4):
                    cc = half * 4 + c
                    TR(PS[b][:, c * 128:(c + 1) * 128], xT[:, cc, tt * 128:(tt + 1) * 128], ident_f[:], [xk(cc, tt // 4)], [pk(b)])
                CP('act' if half == 0 else 'dve', st[:, half * 512:(half + 1) * 512], PS[b][:], [pk(b)], [('xo', slot, half)])
            o = DMA(y_d[s, tt * 128:(tt + 1) * 128, :], st, ('yst', slot), r=[('xo', slot, 0), ('xo', slot, 1)], bar=True)
            outs.append(o)
        P.barrier()
        return outs

    def swap_x(s_st, s_ld):
        P.barrier()
        outs = []
        for tt in range(16):
            slot = tt % 4
            if s_st is not None:
                st = aview(slot * 4096, [128, D], F32)
                for half in range(2):
                    b = bank()
                    for c in range(4):
                        cc = half * 4 + c
                        TR(PS[b][:, c * 128:(c + 1) * 128], xT[:, cc, tt * 128:(tt + 1) * 128], ident_f[:], [xk(cc, tt // 4)], [pk(b)])
                    CP('act' if half == 0 else 'dve', st[:, half * 512:(half + 1) * 512], PS[b][:], [pk(b)], [('xo', slot, half)])
                outs.append(DMA(y_d[s_st, tt * 128:(tt + 1) * 128, :], st, ('yst', slot), r=[('xo', slot, 0), ('xo', slot, 1)], bar=True))
            if s_ld is not None:
                st = aview(16384 + slot * 4096, [128, D], F32)
                k = ('xst', slot)
                DMA(st, x_d[s_ld, tt * 128:(tt + 1) * 128, :], k, w=[k], bar=True)
                for half in range(2):
                    b = bank()
                    for c in range(4):
                        cc = half * 4 + c
                        TR(PS[b][:, c * 128:(c + 1) * 128], st[:, cc * 128:(cc + 1) * 128], ident_f[:], [k], [pk(b)])
                    CP('dve' if half == 0 else 'act', xT[:, half * 4:(half + 1) * 4, tt * 128:(tt + 1) * 128],
                       PS[b][:].rearrange("p (c t) -> p c t", t=128), [pk(b)], [xk(half * 4 + c, tt // 4) for c in range(4)])
        P.barrier()
        return outs


    def ffn(l):
        rmsnorm(gffn, l)
        act = aview(0, [128, 11, S], BF16)
        sg = aview(44 * 1024, [128, 4, 512], BF16)
        wgu = ffn_wgu_d[l].rearrange("(c p) n -> p c n", p=128)
        wd = ffn_wd_d[l].rearrange("(c p) n -> p c n", p=128)
        specs = []
        for part in range(2):
            for jj in range(11):
                j = part * 11 + jj
                specs.append(([(wgu[:, :, j * 128:(j + 1) * 128], 0), (wgu[:, :, DFF + j * 128:DFF + (j + 1) * 128], 128)], 8, 256))
            for n in range(8):
                specs.append(([(wd[:, part * 11:(part + 1) * 11, n * 128:(n + 1) * 128], 0)], 11, 128))
        ws = WStream(specs, ['pool', 'dve'])
        for part in range(2):
            for jj in range(11):
                j = part * 11 + jj
                wv, wk = ws.next()
                for pas in range(2):
                    for kc in range(8):
                        for g in range(4):
                            b = pas * 4 + g
                            MM(PS[b][:], wv[:, kc, pas * 128:(pas + 1) * 128], hT[:, kc, g * 512:(g + 1) * 512],
                               kc == 0, kc == 7, [wk, hk(kc, g)], [pk(b)])
                    if pas == 0:
                        for g in range(4):
                            ACT(sg[:, g, :], PS[g][:], AF.Silu, [pk(g)], [('sg', g)])
                    else:
                        for g in range(4):
                            TT('dve', act[:, jj, g * 512:(g + 1) * 512], sg[:, g, :], PS[4 + g][:], ALU.mult,
                               [('sg', g), pk(4 + g)], [('act', jj, g)])
            for n in range(8):
                wv, wk = ws.next()
                b0 = (n % 2) * 4
                for kc in range(11):
                    for g in range(4):
                        MM(PS[b0 + g][:], wv[:, kc, :], act[:, kc, g * 512:(g + 1) * 512], kc == 0, kc == 10,
                           [wk, ('act', kc, g)], [pk(b0 + g)])
                for g in range(4):
                    TT('dve', xT[:, n, g * 512:(g + 1) * 512], PS[b0 + g][:], xT[:, n, g * 512:(g + 1) * 512], ALU.add,
                       [pk(b0 + g), xk(n, g)], [xk(n, g)])

    def out_specs(w_d):
        wo = w_d.rearrange("(c p) n -> p c n", p=128)
        return [([(wo[:, :, nb * 256:(nb + 1) * 256], 0)], 8, 256) for nb in range(4)]

    def out_proj(ws, src, srckey, groups):
        ng = len(groups)
        for nb in range(4):
            wv, wk = ws.next()
            for cc in range(2):
                n = nb * 2 + cc
                bs = [bank() for _ in groups]
                for kc in range(8):
                    for gi, g in enumerate(groups):
                        MM(PS[bs[gi]][:], wv[:, kc, cc * 128:(cc + 1) * 128], src[:, kc, g * 512:(g + 1) * 512],
                           kc == 0, kc == 7, [wk, srckey(kc, g)], [pk(bs[gi])])
                for gi, g in enumerate(groups):
                    TT('dve', xT[:, n, g * 512:(g + 1) * 512], PS[bs[gi]][:], xT[:, n, g * 512:(g + 1) * 512], ALU.add,
                       [pk(bs[gi]), xk(n, g)], [xk(n, g)])

    def gla(l, j):
        rmsnorm(gattn, l)
        K1 = 1024
        qT = aview(0, [128, 4, 512], BF16)
        kT = aview(4 * K1, [128, 4, 512], BF16)
        ktok = aview(8 * K1, [128, 4, 512], BF16)
        vv = aview(12 * K1, [128, 4, 1024], BF16)
        gsr = aview(20 * K1, [128, 4, 1024], BF16)
        glrT = aview(28 * K1, [32, 512], BF16)
        Eb = aview(29 * K1, [128, 512], F32)
        la = aview(31 * K1, [128, 512], F32)
        ebT = aview(33 * K1, [128, 512], F32)
        enbT = aview(35 * K1, [128, 512], F32)
        es = aview(37 * K1, [128, 512], F32)
        qf = aview(39 * K1, [128, 4, 128], BF16)
        qc1 = aview(40 * K1, [128, 4, 128], BF16)
        qc2 = aview(41 * K1, [128, 4, 128], BF16)
        ktl = aview(42 * K1, [128, 4, 128], BF16)
        ks = aview(43 * K1, [128, 512], BF16)
        scT = aview(44 * K1, [128, 4, 128], BF16)
        S32 = aview(45 * K1, [128, 4, 256], F32)
        Sa = aview(49 * K1, [128, 4, 256], BF16)
        Sb = aview(51 * K1, [128, 4, 256], BF16)
        yy = aview(53 * K1, [128, 1024], BF16)
        junk = aview(55 * K1, [128, 256], BF16)
        ss4 = aview(55 * K1 + 512, [128, 4], F32)
        t4 = aview(55 * K1 + 544, [128, 4], F32)
        rs4 = aview(55 * K1 + 576, [128, 4], F32)
        gfull = aview(56 * K1, [128, 1024], BF16)
        W2b = aview(58 * K1, [32, 512], BF16)
        silt = aview(62 * K1, [128, 2, 256], BF16)
        ebTs = [ebT, aview(59 * K1, [128, 512], F32)]
        scTs = [scT, aview(61 * K1, [128, 4, 128], BF16)]
        kss = [ks, aview(63 * K1, [128, 512], BF16)]
        qc1s = [qc1, aview(64 * K1, [128, 4, 128], BF16)]
        qc2s = [qc2, aview(65 * K1, [128, 4, 128], BF16)]

        win = gla_w_in_d[j].rearrange("(c p) n -> p c n", p=128)
        DMA(W2s[0:16, :], gla_w2_d[j], 'w2s', w=['w2s'])
        DMA(W2s[16:17, :], gla_b_d[j:j + 1, :], 'w2s', r=['w2s'], w=['w2s'])
        CP('pool', W2b[0:17, :], W2s[0:17, :], ['w2s'], ['w2b'])
        DMA(grow[:], gla_ng_d[j:j + 1, :], 'grow', w=['grow'])
        b = bank()
        MM(PS[b][:, 0:256], ones_f[:], grow[:], True, True, ['grow'], [pk(b)], cr=['ones_f'])
        for h in range(4):
            CP('dve', gfull[:, h * 256:(h + 1) * 256], PS[b][:, 0:256], [pk(b)], [('gfull', h)])
        MS('pool', glrT[:], 1.0, ['glrT'])
        for p_ in range(2):
            MS('pool', qc1s[p_], 0.0, [('qc1', p_)])
            MS('pool', qc2s[p_], 0.0, [('qc2', p_)])
        MS('dve', S32[:], 0.0, [('S32', h) for h in range(4)])
        MS('pool', Sb[:], 0.0, ['Sb'])
        scale = 128 ** -0.5
        specs = []
        for qt in range(4):
            for blk in range(4):
                specs.append(([(win[:, :, blk * 256:(blk + 1) * 256], 0)], 8, 256))
            specs.append(([(win[:, :, 3072:3088], 0)], 8, 16))
            for blk in range(10):
                c0 = 512 + blk * 256
                specs.append(([(win[:, :, c0:c0 + 256], 0)], 8, 256))
            specs += out_specs(gla_wo_d[j])
        ws = WStream(specs, ['dve', 'pool', 'act'])

        for qt in range(4):
            t0q = qt * 512
            for blk in range(4):
                wv, wk = ws.next()
                for cc in range(2):
                    n = (blk % 2) * 2 + cc
                    b = bank()
                    for kc in range(8):
                        MM(PS[b][:], wv[:, kc, cc * 128:(cc + 1) * 128], hT[:, kc, t0q:t0q + 512], kc == 0, kc == 7,
                           [wk, hk(kc, qt)], [pk(b)])
                    dst = qT if blk < 2 else kT
                    CP('act' if cc == 0 else 'dve', dst[:, n, :], PS[b][:], [pk(b)], [('qT' if blk < 2 else 'kT', n)])
            wv, wk = ws.next()
            b = bank()
            for kc in range(8):
                MM(PS[b][0:16, :], wv[:, kc, :], hT[:, kc, t0q:t0q + 512], kc == 0, kc == 7, [wk, hk(kc, qt)], [pk(b)])
            CP('act', glrT[0:16, :], PS[b][0:16, :], [pk(b)], ['glrT'])
            for blk in range(10):
                c0 = 512 + blk * 256
                wv, wk = ws.next()
                for tp in range(2):
                    b = bank()
                    for th in range(2):
                        tt = tp * 2 + th
                        for kc in range(8):
                            MM(PS[b][:, th * 256:(th + 1) * 256], hT[:, kc, t0q + tt * 128:t0q + (tt + 1) * 128], wv[:, kc, :],
                               kc == 0, kc == 7, [wk, hk(kc, qt)], [pk(b)])
                    pv = PS[b][:].rearrange("p (a n) -> p a n", n=256)
                    if blk < 2:
                        CP('dve', ktok[:, tp * 2:tp * 2 + 2, blk * 256:(blk + 1) * 256], pv, [pk(b)], [('ktok', tp * 2), ('ktok', tp * 2 + 1)])
                    elif blk < 6:
                        vb = blk - 2
                        CP('act', vv[:, tp * 2:tp * 2 + 2, vb * 256:(vb + 1) * 256], pv, [pk(b)], [('v', tp * 2, vb), ('v', tp * 2 + 1, vb)])
                    else:
                        rb = blk - 6
                        ACT(silt, pv, AF.Silu, [pk(b)], ['silt'])
                        for th in range(2):
                            tt = tp * 2 + th
                            TT('dve', gsr[:, tt, rb * 256:(rb + 1) * 256], silt[:, th, :], gfull[:, rb * 256:(rb + 1) * 256], ALU.mult,
                               ['silt', ('gfull', rb)], [('gsr', tt, rb)])
            def prepA_pe(tt):
                t0 = tt * 128
                MM(PS[0][:], glrT[0:17, t0:t0 + 128], W2b[0:17, :], True, True, ['glrT', 'w2b'], [pk(0)])

            def prepA_act(tt):
                ACT(Eb, PS[0][:], AF.Exp, [pk(0)], ['Eb'], scale=-1.0)
                ACT(la, Eb, AF.Ln, ['Eb'], ['la'], bias=onecol[:])

            def prepB_pe(tt):
                for h in range(4):
                    MM(PS[1][:, h * 128:(h + 1) * 128], la[:, h * 128:(h + 1) * 128], triS_f[:], True, True, ['la'], [pk(1)], cr=['triS_f'])
                MM(PS[0][:], triR_f[:], la, True, True, ['la'], [pk(0)], cr=['triR_f'])

            def prepB_act(tt):
                p = tt % 2
                ACT(ebTs[p], PS[1][:], AF.Exp, [pk(1)], [('ebT', p)])
                ACT(enbT, PS[1][:], AF.Exp, [pk(1)], ['enbT'], scale=-1.0)
                ACT(es, PS[0][:], AF.Exp, [pk(0)], ['es'])

            def prepB_dve(tt):
                t0 = tt * 128
                p = tt % 2
                STT(qf, qT[:, :, t0:t0 + 128], scale, ebTs[p].rearrange("p (h t) -> p h t", t=128), ALU.mult, ALU.mult,
                    [('qT', n) for n in range(4)] + [('ebT', p)], ['qf'])
                CP('pool', qc1s[p][:, :, 0:64], qf[:, :, 0:64], ['qf'], [('qc1', p)])
                CP('pool', qc2s[p][:, :, 64:128], qf[:, :, 64:128], ['qf'], [('qc2', p)])
                TT('dve', ktl, kT[:, :, t0:t0 + 128], enbT.rearrange("p (h t) -> p h t", t=128), ALU.mult,
                   [('kT', n) for n in range(4)] + ['enbT'], ['ktl'])
                TT('dve', kss[p], ktok[:, tt, :], es, ALU.mult, [('ktok', tt), 'es'], [('ks', p)])

            def prepC_pe(tt):
                for h in range(4):
                    MM(PS[3][:, h * 128:(h + 1) * 128], ktl[:, h, :], qf[:, h, :], True, True, ['ktl', 'qf'], [pk(3)])

            def prepC_dve(tt):
                p = tt % 2
                TT('dve', scTs[p], PS[3][:].rearrange("p (h t) -> p h t", t=128), maskG_b[:], ALU.mult, [pk(3)], [('scT', p)])

            def hv(h):
                return (4 + h // 2, slice((h % 2) * 256, (h % 2 + 1) * 256), 6 + h // 2,
                        slice((h % 2) * 256, (h % 2 + 1) * 256), slice(h * 256, (h + 1) * 256), slice(h * 128, (h + 1) * 128))

            def tile_step(tt, nx):
                p = tt % 2
                t0 = tt * 128
                ebT, ks, scT, qc1, qc2 = ebTs[p], kss[p], scTs[p], qc1s[p], qc2s[p]
                for h in range(4):
                    ob, osl, tb, tsl, vsl, ksl = hv(h)
                    MM(PS[ob][:, osl], scT[:, h, :], vv[:, tt, vsl], h % 2 == 0, False, [('scT', p), ('v', tt, h)], [pk(ob)])
                for h in range(4):
                    ob, osl, tb, tsl, vsl, ksl = hv(h)
                    MM(PS[tb][:, tsl], ks[0:64, ksl], vv[0:64, tt, vsl], True, True, [('ks', p), ('v', tt, h)], [pk(tb)])
                for h in range(4):
                    ob, osl, tb, tsl, vsl, ksl = hv(h)
                    MM(PS[ob][:, osl], qc1[:, h, :], Sb[:, h, :], False, False, [('qc1', p), 'Sb'], [pk(ob)])
                if nx:
                    prepA_pe(tt + 1)
                for h in range(4):
                    ob, osl, tb, tsl, vsl, ksl = hv(h)
                    STT(S32[:, h, :], S32[:, h, :], ebT[:, h * 128 + 63:h * 128 + 64], PS[tb][:, tsl], ALU.mult, ALU.add,
                        [('S32', h), ('ebT', p), pk(tb)], [('S32', h)])
                CP('act', Sa[:], S32[:], [('S32', h) for h in range(4)], ['Sa'])
                if nx:
                    prepA_act(tt + 1)
                for h in range(4):
                    ob, osl, tb, tsl, vsl, ksl = hv(h)
                    MM(PS[tb][:, tsl], ks[64:128, ksl], vv[64:128, tt, vsl], True, True, [('ks', p), ('v', tt, h)], [pk(tb)])
                for h in range(4):
                    ob, osl, tb, tsl, vsl, ksl = hv(h)
                    MM(PS[ob][:, osl], qc2[:, h, :], Sa[:, h, :], False, h % 2 == 1, [('qc2', p), 'Sa'], [pk(ob)])
                if nx:
                    prepB_pe(tt + 1)
                for h in range(4):
                    ob, osl, tb, tsl, vsl, ksl = hv(h)
                    STT(S32[:, h, :], S32[:, h, :], ebT[:, h * 128 + 127:h * 128 + 128], PS[tb][:, tsl], ALU.mult, ALU.add,
                        [('S32', h), ('ebT', p), pk(tb)], [('S32', h)])
                CP('act', Sb[:], S32[:], [('S32', h) for h in range(4)], ['Sb'])
                for h in range(4):
                    ob, osl, tb, tsl, vsl, ksl = hv(h)
                    ACT(junk, PS[ob][:, osl], AF.Square, [pk(ob)], ['junk', ('ss4', h)], accum_out=ss4[:, h:h + 1])
                ACT(t4, ss4, AF.Ln, [('ss4', h) for h in range(4)], ['t4'], scale=1.0 / 256.0, bias=epscol[:])
                ACT(rs4, t4, AF.Exp, ['t4'], ['rs4'], scale=-0.5)
                if nx:
                    prepB_act(tt + 1)
                for h in range(4):
                    ob, osl, tb, tsl, vsl, ksl = hv(h)
                    STT(yy[:, h * 256:(h + 1) * 256], PS[ob][:, osl], rs4[:, h:h + 1], gsr[:, tt, h * 256:(h + 1) * 256], ALU.mult, ALU.mult,
                        [pk(ob), 'rs4', ('gsr', tt, h)], [('yy', h)])
                ybank = PS[2][:].bitcast(BF16)
                for c in range(8):
                    TR(ybank[:, c * 128:(c + 1) * 128], yy[:, c * 128:(c + 1) * 128], ident_b[:], [('yy', c // 2)], [pk(2)])
                CP('act', hT[:, :, t0q + t0:t0q + t0 + 128], ybank.rearrange("p (c t) -> p c t", t=128), [pk(2)],
                   [hk(c, qt) for c in range(8)])
                if nx:
                    prepB_dve(tt + 1)
                    prepC_pe(tt + 1)
                    prepC_dve(tt + 1)

            prepA_pe(0)
            prepA_act(0)
            prepB_pe(0)
            prepB_act(0)
            prepB_dve(0)
            prepC_pe(0)
            prepC_dve(0)
            for tt in range(4):
                tile_step(tt, tt + 1 < 4)
            out_proj(ws, hT, hk, [qt])

    def sb(l, j):
        rmsnorm(gattn, l)
        K1 = 1024
        oT = aview(0, [128, 8, S], BF16)
        qTcs = [aview(32 * K1, [128, S], BF16), wst[2][:, 0:1024].bitcast(BF16)]
        kTcs = [aview(36 * K1, [128, S], BF16), wst[2][:, 1024:2048].bitcast(BF16)]
        XK = [('wst', 2, 0), ('wst', 2, 1)]
        vpad = aview(40 * K1, [128, 16, 2, 128], BF16)
        Ef = [aview(48 * K1 + i * 2048, [128, 512], F32) for i in range(2)]
        spb = [aview(52 * K1 + i * 1024, [128, 512], BF16) for i in range(2)] + [aview(64 * K1, [128, 512], BF16)]
        wTb = [aview(54 * K1 + i * 1024, [128, 512], BF16) for i in range(2)]
        acc32 = aview(56 * K1, [128, 512], F32)
        accb = [aview(58 * K1 + i * 1024, [128, 512], BF16) for i in range(2)]
        sqb = [aview(60 * K1 + i * 1024, [128, 512], BF16) for i in range(2)]
        lnt = aview(62 * K1, [128, 512], F32)
        win = sb_w_in_d[j].rearrange("(c p) n -> p c n", p=128)
        MS('pool', vpad[:], 0.0, [('vpad', tq_, h_) for tq_ in range(4) for h_ in range(2)])
        nrm = [0]
        specs = []
        for c in range(8):
            specs.append(([(win[:, :, c * 128:(c + 1) * 128], 0), (win[:, :, D + c * 128:D + (c + 1) * 128], 128)], 8, 256))
            specs.append(([(win[:, :, 2 * D + c * 128:2 * D + (c + 1) * 128], 0)], 8, 128))
        specs += out_specs(sb_wo_d[j])
        ws = WStream(specs, ['pool', 'dve'], look=1, nst=2)

        def qknorm(bs, dst, dkey, gvec):
            for g in range(4):
                i = nrm[0] % 2
                nrm[0] += 1
                ACT(sqb[i], PS[bs[g]][:], AF.Square, [pk(bs[g])], [('sqb', i)])
                cb = bank('C')
                MM(PS[cb][:], blk_b[:], sqb[i], True, True, [('sqb', i)], [pk(cb)], cr=['blk_b'])
                ACT(lnt, PS[cb][:], AF.Ln, [pk(cb)], ['lnt'], scale=1.0 / 64.0, bias=epscol[:])
                ACT(lnt, lnt, AF.Exp, ['lnt'], ['lnt'], scale=-0.5)
                STT(dst[:, g * 512:(g + 1) * 512], PS[bs[g]][:], gvec[:, j:j + 1], lnt, ALU.mult, ALU.mult,
                    [pk(bs[g]), 'lnt'], [dkey + (g,)])

        def make_units(wq, wqk_key, par):
            inj = {}
            for u in range(8):
                which, g = u // 4, u % 4
                base = 2 + u * 8
                dst = qTcs[par] if which == 0 else kTcs[par]
                dkey = ('qTc', par, g) if which == 0 else ('kTc', par, g)
                gvec = gq if which == 0 else gk
                i = u % 2

                def f_proj(which=which, g=g):
                    for kc in range(8):
                        MM(PS[6][:], wq[:, kc, which * 128:(which + 1) * 128], hT[:, kc, g * 512:(g + 1) * 512],
                           kc == 0, kc == 7, [wqk_key, hk(kc, g)], [pk(6)])

                def f_sq(i=i):
                    ACT(sqb[i], PS[6][:], AF.Square, [pk(6)], [('sqb', i)])

                def f_ss(i=i):
                    MM(PS[7][:], blk_b[:], sqb[i], True, True, [('sqb', i)], [pk(7)], cr=['blk_b'])

                def f_ln():
                    ACT(lnt, PS[7][:], AF.Ln, [pk(7)], ['lnt'], scale=1.0 / 64.0, bias=epscol[:])
                    ACT(lnt, lnt, AF.Exp, ['lnt'], ['lnt'], scale=-0.5)

                def f_stt(dst=dst, dkey=dkey, g=g, gvec=gvec):
                    STT(dst[:, g * 512:(g + 1) * 512], PS[6][:], gvec[:, j:j + 1], lnt, ALU.mult, ALU.mult,
                        [pk(6), 'lnt'], [dkey] + (XK if par == 1 else []))

                for off, f in enumerate([f_proj, f_sq, f_ss, f_ln, f_stt]):
                    inj.setdefault(base + off, []).append(f)
            return inj

        for c in range(8):
            par = c % 2
            qTc, kTc = qTcs[par], kTcs[par]

            def qkproj(which):
                bs = [0, 1, 2, 3]
                for kc in range(8):
                    for g in range(4):
                        MM(PS[bs[g]][:], wqk[:, kc, which * 128:(which + 1) * 128], hT[:, kc, g * 512:(g + 1) * 512],
                           kc == 0, kc == 7, [wqkk, hk(kc, g)], [pk(bs[g])])
                return bs

            def vproj(tqs):
                for tq in tqs:
                    b = 4 + tq % 2
                    for th in range(4):
                        tt = tq * 4 + th
                        for kc in range(8):
                            MM(PS[b][:, th * 128:(th + 1) * 128], hT[:, kc, tt * 128:(tt + 1) * 128], wvv[:, kc, :],
                               kc == 0, kc == 7, [wvk, hk(kc, tq)], [pk(b)])
                    pv = PS[b][:].rearrange("p (a n) -> p a n", n=128)
                    CP('dve', vpad[:, tq * 4:tq * 4 + 4, 0, 0:64], pv[:, :, 0:64], [pk(b)], [('vpad', tq, 0)])
                    CP('dve', vpad[:, tq * 4:tq * 4 + 4, 1, 64:128], pv[:, :, 64:128], [pk(b)], [('vpad', tq, 1)])

            if c == 0:
                wqk, wqkk = ws.next()
                bs = qkproj(0)
                wvv, wvk = ws.next()
                vproj([0, 1])
                qknorm(bs, qTc, ('qTc', par), gq)
                bs = qkproj(1)
                vproj([2, 3])
                qknorm(bs, kTc, ('kTc', par), gk)
            else:
                wvv, wvk = ws.next()
                vproj([0, 1, 2, 3])
            inj = {}
            if c < 7:
                wqn, wqnk = ws.next()
                inj = make_units(wqn, wqnk, 1 - par)
            tiles = []
            for qg in range(4):
                for h in range(2):
                    kbs = list(reversed(range(4 * qg + 4)))
                    for ti, kb in enumerate(kbs):
                        tiles.append(dict(qg=qg, h=h, kb=kb, first=(ti == 0), last=(ti == len(kbs) - 1),
                                          firstq=(h == 0 and ti == 0), lastq=(h == 1 and ti == len(kbs) - 1)))
            nt = len(tiles)
            obank = {}

            def stageA(i):
                t = tiles[i]
                qg, h, kb = t['qg'], t['h'], t['kb']
                di = kb - 4 * qg
                q0 = max(di, 0) * 128
                nq = 512 - q0
                t['q0'], t['nq'], t['di'] = q0, nq, di
                hp = slice(h * 64, (h + 1) * 64)
                zb = bank('A')
                t['zb'] = zb
                e = i % 2
                MM(PS[zb][:, 0:nq], kTc[hp, kb * 128:(kb + 1) * 128], qTc[hp, qg * 512 + q0:(qg + 1) * 512], True, True,
                   [('kTc', par, kb // 4), ('qTc', par, qg)] + (XK if par == 1 else []), [pk(zb)])
                ACT(Ef[e][:, 0:nq], PS[zb][:, 0:nq], AF.Exp, [pk(zb)], [('Ef', e)])

            def stageA2(i):
                t = tiles[i]
                nq, di = t['nq'], t['di']
                e = i % 2
                p3 = i % 3
                ACT(spb[p3][:, 0:nq], Ef[e][:, 0:nq], AF.Ln, [('Ef', e)], [('spb', p3)], bias=onecol[:])
                if di >= 0:
                    TT('pool', spb[p3][:, 0:128], spb[p3][:, 0:128], mask01_b[:], ALU.mult, [('spb', p3)], [('spb', p3)])

            def stageB(i):
                t = tiles[i]
                qg, h, kb, q0, nq, di, zb = t['qg'], t['h'], t['kb'], t['q0'], t['nq'], t['di'], t['zb']
                e = i % 2
                p3 = i % 3
                last_is_suffix = t['first'] and di < 0
                MM(PS[zb][:, 0:nq], negU_b[:], spb[p3][:, 0:nq], False, last_is_suffix, [('spb', p3)], [pk(zb)], cr=['negU_b'])
                if not t['first']:
                    MM(PS[zb][:, 0:nq], negO_b[:], accb[(i + 1) % 2][:, q0:512], False, di < 0, [('accb', (i + 1) % 2)], [pk(zb)], cr=['negO_b'])
                if di >= 0:
                    MM(PS[zb][:, 0:128], ident_b[:], negbig_b[:], False, True, [], [pk(zb)], cr=['ident_b', 'negbig_b'])
                ACT(wTb[e][:, 0:nq], PS[zb][:, 0:nq], AF.Exp, [pk(zb)], [('wTb', e)])
                if t['first']:
                    MS('dve', acc32, 0.0, ['acc32'])
                if not t['last']:
                    TT('dve', acc32[:, q0:512], acc32[:, q0:512], spb[p3][:, 0:nq], ALU.add, ['acc32', ('spb', p3)], ['acc32'])
                    CP('dve', accb[i % 2], acc32, ['acc32'], [('accb', i % 2)])

            def stageC(i):
                t = tiles[i]
                qg, h, kb, q0, nq = t['qg'], t['h'], t['kb'], t['q0'], t['nq']
                e = i % 2
                if t['firstq']:
                    ob = bank('B')
                    obank[qg] = ob
                    MM(PS[ob][:], zeros_b[:, 0:128], zeros_b[:], True, False, [], [pk(ob)], cr=['zeros_b'])
                ob = obank[qg]
                MM(PS[ob][:, q0:512], vpad[:, kb, h, :], wTb[e][:, 0:nq], False, t['lastq'], [('vpad', kb // 4, h), ('wTb', e)], [pk(ob)])
                if t['lastq']:
                    CP('dve', oT[:, c, qg * 512:(qg + 1) * 512], PS[ob][:], [pk(ob)], [('oT', c, qg)])

            for step in range(nt + 3):
                for f in inj.get(step, ()):
                    f()
                if step < nt:
                    stageA(step)
                if 0 <= step - 2 < nt:
                    stageB(step - 2)
                if step < nt:
                    stageA2(step)
                if 0 <= step - 3 < nt:
                    stageC(step - 3)
        out_proj(ws, oT, lambda kc, g: ('oT', kc, g), [0, 1, 2, 3])

    finals = []
    for s in range(nseq):
        if s == 0:
            swap_x(None, 0)
        for l in layers:
            if l % 2 == 0:
                gla(l, l // 2)
            else:
                sb(l, l // 2)
            ffn(l)
        finals += swap_x(s, s + 1 if s + 1 < nseq else None)
    P.emit(final_wait_ops=finals)
    return nc, P


_CACHE = {}


def kernel(**inputs):
    x = np.ascontiguousarray(inputs['x'], dtype=np.float32)
    B = x.shape[0]
    nseq = B // NCORES
    key = (nseq,)
    if key not in _CACHE:
        _CACHE[key] = build(nseq, list(range(DEPTH)))[0]
    nc = _CACHE[key]
    names = ['attn_norm_g', 'ffn_norm_g', 'gla_w_in', 'gla_w_gate2', 'gla_b_gate', 'gla_norm_g', 'gla_w_out',
             'sb_w_in', 'sb_q_norm_g', 'sb_k_norm_g', 'sb_w_out', 'ffn_w_gate_up', 'ffn_w_down']
    shared = {n: np.ascontiguousarray(inputs[n], dtype=np.float32) for n in names}
    in_maps = []
    for c in range(NCORES):
        m = dict(shared)
        m['x'] = x[c * nseq:(c + 1) * nseq]
        in_maps.append(m)
    res = run_bass_kernel_spmd(nc, in_maps, core_ids=list(range(NCORES)))
    return np.concatenate([r['y'] for r in res.results], axis=0).astype(np.float32)
```

```python
import contextlib
import numpy as np
import concourse.bass as bass
import concourse.mybir as mybir
from concourse.bass_utils import run_bass_kernel_spmd

F32 = mybir.dt.float32
BF16 = mybir.dt.bfloat16
AF = mybir.ActivationFunctionType
ALU = mybir.AluOpType

D = 1024
S = 2048
DFF = 2816
KD = 512
GIN = 3088
DEPTH = 4
NCORES = 8
EPS = 1e-6


class Prog:
    def __init__(self, nc):
        self.nc = nc
        self.ops = []
        self.lw = {}
        self.rd = {}
        self.last_bar = {}
        self.bar_dmas = []
        self.bar_deps = set()
        self.bar_pending = set()
        self.const_keys = []
        self.const_done = set()

    def barrier(self):
        self.bar_deps = set(self.last_bar.values()) | set(self.bar_dmas)
        self.bar_dmas = []
        self.bar_pending = {'pe', 'act', 'dve', 'pool', 'sp'}

    def op(self, eng, fn, reads=(), writes=(), creads=(), dma=None, bar=None):
        if bar is None:
            bar = dma is None
        raw = set()
        oth = set()
        lw = self.lw
        for k in reads:
            w = lw.get(k)
            if w is not None:
                raw.add(w)
        for k in creads:
            w = lw.get(k)
            if w is not None:
                raw.add(w)
        for k in writes:
            w = lw.get(k)
            if w is not None:
                oth.add(w)
            r = self.rd.get(k)
            if r:
                oth.update(r.values())
        i = len(self.ops)
        deps = set()
        ops = self.ops
        for d in raw:
            de = ops[d]
            if de[3] is None and dma is None and de[0] == eng and eng == 'pe':
                continue
            deps.add(d)
        for d in oth:
            de = ops[d]
            if de[3] is None and dma is None and de[0] == eng and eng == 'pe':
                continue
            deps.add(d)
        if dma is None and self.const_keys and eng not in self.const_done:
            self.const_done.add(eng)
            for k in self.const_keys:
                w = lw.get(k)
                if w is not None and not (ops[w][3] is None and ops[w][0] == eng and eng == 'pe'):
                    deps.add(w)
        if bar and eng in self.bar_pending:
            self.bar_pending.discard(eng)
            for d in self.bar_deps:
                de = ops[d]
                if de[3] is None and dma is None and de[0] == eng and eng == 'pe':
                    continue
                deps.add(d)
        ops.append((eng, fn, deps, dma))
        for k in reads:
            self.rd.setdefault(k, {})[(eng, i if dma is not None else -1)] = i
        for k in writes:
            lw[k] = i
            self.rd[k] = {}
        if bar:
            if dma is None:
                self.last_bar[eng] = i
            else:
                self.bar_dmas.append(i)
        return i

    def emit(self, final_wait_ops=()):
        nc = self.nc
        ops = self.ops
        n = len(ops)
        needed = [False] * n
        for (eng, fn, deps, dma) in ops:
            for d in deps:
                needed[d] = True
        for d in final_wait_ops:
            needed[d] = True
        stack = contextlib.ExitStack()
        engsem = {}
        for e in ('pe', 'act', 'dve', 'pool', 'sp'):
            engsem[e] = stack.enter_context(nc.semaphore('s_' + e))
        dmasem = {}
        for (eng, fn, deps, dma) in ops:
            if dma is not None and dma not in dmasem:
                dmasem[dma] = stack.enter_context(nc.semaphore('d_%d' % len(dmasem)))
        tok = [None] * n
        cnt = {}
        for i, (eng, fn, deps, dma) in enumerate(ops):
            if dma is not None:
                cnt[dma] = cnt.get(dma, 0) + 16
                tok[i] = (dmasem[dma], cnt[dma], dma)
            elif needed[i]:
                cnt[eng] = cnt.get(eng, 0) + 1
                tok[i] = (engsem[eng], cnt[eng], eng)
        self.maxcnt = dict(cnt)
        per = {e: [] for e in ('pe', 'act', 'dve', 'pool', 'sp')}
        for i, o in enumerate(ops):
            per[o[0]].append(i)
        fw = list(final_wait_ops)

        def run(ename, e):
            waited = {}
            for i in per[ename]:
                eng, fn, deps, dma = ops[i]
                for d in sorted(deps):
                    s, v, key = tok[d]
                    if waited.get(key, 0) < v:
                        e.wait_ge(s, v)
                        waited[key] = v
                ins = fn(e)
                if dma is not None:
                    ins.then_inc(tok[i][0], 16)
                elif needed[i]:
                    ins.then_inc(tok[i][0], 1)
            if ename == 'sp':
                for d in fw:
                    s, v, key = tok[d]
                    if waited.get(key, 0) < v:
                        e.wait_ge(s, v)
                        waited[key] = v

        with stack:
            with nc.Block() as block:
                @block.tensor
                def _(e):
                    run('pe', e)

                @block.scalar
                def _(e):
                    run('act', e)

                @block.vector
                def _(e):
                    run('dve', e)

                @block.gpsimd
                def _(e):
                    run('pool', e)

                @block.sync
                def _(e):
                    run('sp', e)


ARENA = 66 * 1024
NS = 3
NB = 3
WCAP = 2048


def build(nseq, layers):
    nc = bass.Bass("TRN2", target_bir_lowering=False)
    P = Prog(nc)

    def din(name, shape):
        return nc.dram_tensor(name, list(shape), F32, kind="ExternalInput").ap()

    x_d = din("x", [nseq, S, D])
    attn_g_d = din("attn_norm_g", [DEPTH, D])
    ffn_g_d = din("ffn_norm_g", [DEPTH, D])
    gla_w_in_d = din("gla_w_in", [2, D, GIN])
    gla_w2_d = din("gla_w_gate2", [2, 16, KD])
    gla_b_d = din("gla_b_gate", [2, KD])
    gla_ng_d = din("gla_norm_g", [2, 256])
    gla_wo_d = din("gla_w_out", [2, D, D])
    sb_w_in_d = din("sb_w_in", [2, D, 3 * D])
    sb_qg_d = din("sb_q_norm_g", [2, 64])
    sb_kg_d = din("sb_k_norm_g", [2, 64])
    sb_wo_d = din("sb_w_out", [2, D, D])
    ffn_wgu_d = din("ffn_w_gate_up", [DEPTH, D, 2 * DFF])
    ffn_wd_d = din("ffn_w_down", [DEPTH, DFF, D])
    y_d = nc.dram_tensor("y", [nseq, S, D], F32, kind="ExternalOutput").ap()

    xT = nc.alloc_sbuf_tensor("xT", [128, 8, S], F32)
    hT = nc.alloc_sbuf_tensor("hT", [128, 8, S], BF16)
    wst = [nc.alloc_sbuf_tensor("wst%d" % i, [128, WCAP], F32) for i in range(NS)]
    wbf = [nc.alloc_sbuf_tensor("wbf%d" % i, [128, WCAP], BF16) for i in range(NB)]
    arena = nc.alloc_sbuf_tensor("arena", [128, ARENA // 2], BF16)
    ident_f = nc.alloc_sbuf_tensor("ident_f", [128, 128], F32)
    ident_b = nc.alloc_sbuf_tensor("ident_b", [128, 128], BF16)
    ones_b = nc.alloc_sbuf_tensor("ones_b", [128, 128], BF16)
    blk_b = nc.alloc_sbuf_tensor("blk_b", [128, 128], BF16)
    negU_b = nc.alloc_sbuf_tensor("negU_b", [128, 128], BF16)
    negO_b = nc.alloc_sbuf_tensor("negO_b", [128, 128], BF16)
    negbig_b = nc.alloc_sbuf_tensor("negbig_b", [128, 128], BF16)
    mask01_b = nc.alloc_sbuf_tensor("mask01_b", [128, 128], BF16)
    zeros_b = nc.alloc_sbuf_tensor("zeros_b", [128, 512], BF16)
    triS_f = nc.alloc_sbuf_tensor("triS_f", [128, 128], F32)
    triR_f = nc.alloc_sbuf_tensor("triR_f", [128, 128], F32)
    maskG_b = nc.alloc_sbuf_tensor("maskG_b", [128, 4, 128], BF16)
    onecol = nc.alloc_sbuf_tensor("onecol", [128, 1], F32)
    epscol = nc.alloc_sbuf_tensor("epscol", [128, 1], F32)
    gattn = nc.alloc_sbuf_tensor("gattn", [128, DEPTH, 8], F32)
    gffn = nc.alloc_sbuf_tensor("gffn", [128, DEPTH, 8], F32)
    gq = nc.alloc_sbuf_tensor("gq", [128, 2], F32)
    gk = nc.alloc_sbuf_tensor("gk", [128, 2], F32)
    ones_f = nc.alloc_sbuf_tensor("ones_f", [1, 128], F32)
    W2s = nc.alloc_sbuf_tensor("W2s", [32, 512], F32)
    grow = nc.alloc_sbuf_tensor("grow", [1, 256], F32)
    PS = [nc.alloc_psum_tensor("ps%d" % i, [128, 512], F32) for i in range(8)]

    def pk(i):
        return ('ps', i)

    def aview(off, shape, dt):
        nel = 1
        for s_ in shape[1:]:
            nel *= s_
        nb = nel * (4 if dt == F32 else 2)
        assert off % 4 == 0 and off + nb <= ARENA, (off, nb)
        ap = arena[:, off // 2:(off + nb) // 2]
        if dt == F32:
            ap = ap.bitcast(F32)
        if len(shape) == 3:
            ap = ap.rearrange("p (a b) -> p a b", b=shape[2])
        elif len(shape) == 4:
            ap = ap.rearrange("p (a b c) -> p a b c", b=shape[2], c=shape[3])
        if shape[0] != 128:
            ap = ap[0:shape[0]]
        return ap

    def MM(out, lhsT, rhs, start, stop, r, w, cr=()):
        P.op('pe', lambda e: e.matmul(out, lhsT, rhs, start=start, stop=stop, skip_group_check=True), reads=r, writes=w, creads=cr)

    def TR(out, in_, ident, r, w):
        P.op('pe', lambda e: e.transpose(out, in_, ident), reads=r, writes=w)

    def ACT(out, in_, func, r, w, scale=None, bias=None, accum_out=None):
        kw = {}
        if scale is not None:
            kw['scale'] = scale
        if bias is not None:
            kw['bias'] = bias
        if accum_out is not None:
            kw['accum_out'] = accum_out
        P.op('act', lambda e: e.activation(out, in_, func, **kw), reads=r, writes=w)

    def TT(eng, out, in0, in1, op, r, w):
        P.op(eng, lambda e: e.tensor_tensor(out, in0, in1, op), reads=r, writes=w)

    def STT(out, in0, scalar, in1, op0, op1, r, w):
        P.op('dve', lambda e: e.scalar_tensor_tensor(out, in0, scalar, in1, op0, op1), reads=r, writes=w)

    def CP(eng, out, in_, r, w, bar=None):
        if eng == 'act':
            P.op('act', lambda e: e.activation(out, in_, AF.Copy), reads=r, writes=w, bar=bar)
        else:
            P.op(eng, lambda e: e.tensor_copy(out, in_), reads=r, writes=w, bar=bar)

    def MS(eng, ap, val, w, r=()):
        P.op(eng, lambda e: e.memset(ap, val), reads=r, writes=w)

    def ASEL(ap, pattern, cmp, fill, base, cm, key):
        P.op('pool', lambda e: e.affine_select(ap, ap, pattern, cmp, fill, base=base, channel_multiplier=cm), reads=[key], writes=[key])

    def DMA(out, in_, key, r=(), w=(), eng='sp', bar=False, slow=False):
        if slow:
            return P.op(eng, lambda e: e.dma_start(out=out, in_=in_, allow_slow_non_contiguous=True), reads=r, writes=w, dma=key, bar=bar)
        return P.op(eng, lambda e: e.dma_start(out=out, in_=in_), reads=r, writes=w, dma=key, bar=bar)

    MS('pool', ident_f[:], 1.0, ['ident_f'])
    ASEL(ident_f[:], [[-1, 128]], ALU.is_equal, 0.0, 0, 1, 'ident_f')
    MS('pool', ident_b[:], 1.0, ['ident_b'])
    ASEL(ident_b[:], [[-1, 128]], ALU.is_equal, 0.0, 0, 1, 'ident_b')
    MS('pool', ones_b[:], 1.0, ['ones_b'])
    MS('pool', blk_b[:], 1.0, ['blk_b'])
    MS('pool', blk_b[0:64, 64:128], 0.0, ['blk_b'])
    MS('pool', blk_b[64:128, 0:64], 0.0, ['blk_b'])
    MS('pool', negU_b[:], -1.0, ['negU_b'])
    ASEL(negU_b[:], [[-1, 128]], ALU.is_ge, 0.0, 0, 1, 'negU_b')
    MS('pool', negO_b[:], -1.0, ['negO_b'])
    MS('pool', negbig_b[:], -30000.0, ['negbig_b'])
    ASEL(negbig_b[:], [[-1, 128]], ALU.is_ge, 0.0, 0, 1, 'negbig_b')
    MS('pool', mask01_b[:], 1.0, ['mask01_b'])
    ASEL(mask01_b[:], [[1, 128]], ALU.is_gt, 0.0, 0, -1, 'mask01_b')
    MS('pool', zeros_b[:], 0.0, ['zeros_b'])
    MS('pool', triS_f[:], -1.0 / 16.0, ['triS_f'])
    ASEL(triS_f[:], [[1, 128]], ALU.is_ge, 0.0, 0, -1, 'triS_f')
    MS('pool', triS_f[0:64, 64:128], 0.0, ['triS_f'])
    MS('pool', triR_f[:], -1.0 / 16.0, ['triR_f'])
    ASEL(triR_f[:], [[-1, 128]], ALU.is_gt, 0.0, 0, 1, 'triR_f')
    MS('pool', triR_f[64:128, 0:64], 0.0, ['triR_f'])
    MS('pool', maskG_b[:], 1.0, ['maskG_b'])
    ASEL(maskG_b[:], [[0, 4], [1, 128]], ALU.is_ge, 0.0, 0, -1, 'maskG_b')
    MS('pool', maskG_b[0:64, :, 64:128], 0.0, ['maskG_b'])
    MS('pool', onecol[:], 1.0, ['onecol'])
    MS('pool', epscol[:], EPS, ['epscol'])
    MS('pool', ones_f[:], 1.0, ['ones_f'])
    CONSTS = ['ident_f', 'ident_b', 'ones_b', 'blk_b', 'negU_b', 'negO_b', 'negbig_b', 'mask01_b', 'zeros_b',
              'triS_f', 'triR_f', 'maskG_b', 'onecol', 'epscol', 'ones_f', 'gattn', 'gffn', 'gq', 'gk']

    DMA(gattn[:], attn_g_d.rearrange("l (c p) -> p l c", p=128), 'gattn', w=['gattn'], eng='act', slow=True)
    DMA(gffn[:], ffn_g_d.rearrange("l (c p) -> p l c", p=128), 'gffn', w=['gffn'], eng='act', slow=True)
    for j in range(2):
        for hh in range(2):
            DMA(gq[hh * 64:(hh + 1) * 64, j:j + 1], sb_qg_d[j:j + 1, :].rearrange("o d -> d o"), 'gq', r=['gq'], w=['gq'], eng='act')
            DMA(gk[hh * 64:(hh + 1) * 64, j:j + 1], sb_kg_d[j:j + 1, :].rearrange("o d -> d o"), 'gk', r=['gk'], w=['gk'], eng='act')
    P.op('dve', lambda e: e.tensor_scalar(gq[:], gq[:], 0.125, None, ALU.mult), reads=['gq'], writes=['gq'])

    P.const_keys = CONSTS

    rot = {'all': [0, list(range(8))], 'A': [0, [0, 1, 2, 3]], 'B': [0, [4, 5]], 'C': [0, [6, 7]]}

    def bank(pool='all'):
        st = rot[pool]
        b = st[1][st[0] % len(st[1])]
        st[0] += 1
        return b

    wctr = [0, 0]

    def wissue(pieces, KC, N, ceng, nst):
        si = wctr[0] % nst
        bi = wctr[1] % NB
        wctr[0] += 1
        wctr[1] += 1
        assert KC * N <= WCAP
        sv = wst[si][:, 0:KC * N].rearrange("p (c n) -> p c n", n=N)
        keys = []
        for pi, (ap, c0) in enumerate(pieces):
            n = ap.shape[2]
            k = ('wst', si, pi)
            keys.append(k)
            DMA(sv[:, :, c0:c0 + n], ap, k, w=[k])
        bv = wbf[bi][:, 0:KC * N]
        CP(ceng, bv, wst[si][:, 0:KC * N], keys, [('wbf', bi)], bar=False)
        return bv.rearrange("p (c n) -> p c n", n=N), ('wbf', bi)

    class WStream:
        def __init__(self, specs, engs, look=2, nst=NS):
            self.nst = nst
            self.specs = specs
            self.engs = engs
            self.look = look
            self.i = 0
            self.loaded = []

        def next(self):
            while len(self.loaded) < min(len(self.specs), self.i + 1 + self.look):
                k = len(self.loaded)
                pieces, KC, N = self.specs[k]
                self.loaded.append(wissue(pieces, KC, N, self.engs[k % len(self.engs)], self.nst))
            r = self.loaded[self.i]
            self.i += 1
            return r

    def xk(c, g):
        return ('x', c, g)

    def hk(c, g):
        return ('h', c, g)

    def rmsnorm(gt, l):
        P.barrier()
        sqs = [aview(ARENA - 24 * 1024 + i * 8192, [128, 8, 512], BF16) for i in range(2)]
        lnts = [aview(ARENA - 8 * 1024 + i * 2048, [128, 512], F32) for i in range(2)]
        rstds = [aview(ARENA - 4 * 1024 + i * 2048, [128, 512], F32) for i in range(2)]
        def sqr(g):
            ts = slice(g * 512, (g + 1) * 512)
            ACT(sqs[g % 2], xT[:, :, ts], AF.Square, [xk(c, g) for c in range(8)], [('n_sq', g % 2)])

        sqr(0)
        for g in range(4):
            i = g % 2
            sq, lnt, rstd = sqs[i], lnts[i], rstds[i]
            ts = slice(g * 512, (g + 1) * 512)
            b = bank()
            for c in range(8):
                MM(PS[b][:], ones_b[:], sq[:, c, :], c == 0, c == 7, [('n_sq', i)], [pk(b)], cr=['ones_b'])
            if g + 1 < 4:
                sqr(g + 1)
            ACT(lnt, PS[b][:], AF.Ln, [pk(b)], [('n_ln', i)], scale=1.0 / D, bias=epscol[:])
            ACT(rstd, lnt, AF.Exp, [('n_ln', i)], [('n_rstd', i)], scale=-0.5)
            for c in range(8):
                STT(hT[:, c, ts], xT[:, c, ts], gt[:, l, c:c + 1], rstd, ALU.mult, ALU.mult,
                    [xk(c, g), ('n_rstd', i)], [hk(c, g)])
        P.barrier()

    def load_x(s):
        P.barrier()
        for tt in range(16):
            slot = tt % 2
            st = aview(slot * 4096, [128, D], F32)
            k = ('xst', slot)
            DMA(st, x_d[s, tt * 128:(tt + 1) * 128, :], k, w=[k], bar=True)
            for half in range(2):
                b = bank()
                for c in range(4):
                    cc = half * 4 + c
                    TR(PS[b][:, c * 128:(c + 1) * 128], st[:, cc * 128:(cc + 1) * 128], ident_f[:], [k], [pk(b)])
                CP('act' if half == 0 else 'dve', xT[:, half * 4:(half + 1) * 4, tt * 128:(tt + 1) * 128],
                   PS[b][:].rearrange("p (c t) -> p c t", t=128), [pk(b)], [xk(half * 4 + c, tt // 4) for c in range(4)])
        P.barrier()

    def store_x(s):
        P.barrier()
        outs = []
        for tt in range(16):
            slot = tt % 2
            st = aview(slot * 4096, [128, D], F32)
            k = ('xst', slot)
            for half in range(2):
                b = bank()
                for c in range(4):
                    cc = half * 4 + c
                    TR(PS[b][:, c * 128:(c + 1) * 128], xT[:, cc, tt * 128:(tt + 1) * 128], ident_f[:], [xk(cc, tt // 4)], [pk(b)])
                CP('act' if half == 0 else 'dve', st[:, half * 512:(half + 1) * 512], PS[b][:], [pk(b)], [('xo', slot, half)])
            o = DMA(y_d[s, tt * 128:(tt + 1) * 128, :], st, ('yst', slot), r=[('xo', slot, 0), ('xo', slot, 1)], bar=True)
            outs.append(o)
        P.barrier()
        return outs

    def swap_x(s_st, s_ld):
        P.barrier()
        outs = []
        for tt in range(16):
            slot = tt % 4
            if s_st is not None:
                st = aview(slot * 4096, [128, D], F32)
                for half in range(2):
                    b = bank()
                    for c in range(4):
                        cc = half * 4 + c
                        TR(PS[b][:, c * 128:(c + 1) * 128], xT[:, cc, tt * 128:(tt + 1) * 128], ident_f[:], [xk(cc, tt // 4)], [pk(b)])
                    CP('act' if half == 0 else 'dve', st[:, half * 512:(half + 1) * 512], PS[b][:], [pk(b)], [('xo', slot, half)])
                outs.append(DMA(y_d[s_st, tt * 128:(tt + 1) * 128, :], st, ('yst', slot), r=[('xo', slot, 0), ('xo', slot, 1)], bar=True))
            if s_ld is not None:
                st = aview(16384 + slot * 4096, [128, D], F32)
                k = ('xst', slot)
                DMA(st, x_d[s_ld, tt * 128:(tt + 1) * 128, :], k, w=[k], bar=True)
                for half in range(2):
                    b = bank()
                    for c in range(4):
                        cc = half * 4 + c
                        TR(PS[b][:, c * 128:(c + 1) * 128], st[:, cc * 128:(cc + 1) * 128], ident_f[:], [k], [pk(b)])
                    CP('dve' if half == 0 else 'act', xT[:, half * 4:(half + 1) * 4, tt * 128:(tt + 1) * 128],
                       PS[b][:].rearrange("p (c t) -> p c t", t=128), [pk(b)], [xk(half * 4 + c, tt // 4) for c in range(4)])
        P.barrier()
        return outs


    def ffn(l):
        rmsnorm(gffn, l)
        act = aview(0, [128, 11, S], BF16)
        sg = aview(44 * 1024, [128, 4, 512], BF16)
        wgu = ffn_wgu_d[l].rearrange("(c p) n -> p c n", p=128)
        wd = ffn_wd_d[l].rearrange("(c p) n -> p c n", p=128)
        specs = []
        for part in range(2):
            for jj in range(11):
                j = part * 11 + jj
                specs.append(([(wgu[:, :, j * 128:(j + 1) * 128], 0), (wgu[:, :, DFF + j * 128:DFF + (j + 1) * 128], 128)], 8, 256))
            for n in range(8):
                specs.append(([(wd[:, part * 11:(part + 1) * 11, n * 128:(n + 1) * 128], 0)], 11, 128))
        ws = WStream(specs, ['pool', 'dve'])
        for part in range(2):
            for jj in range(11):
                j = part * 11 + jj
                wv, wk = ws.next()
                for pas in range(2):
                    for kc in range(8):
                        for g in range(4):
                            b = pas * 4 + g
                            MM(PS[b][:], wv[:, kc, pas * 128:(pas + 1) * 128], hT[:, kc, g * 512:(g + 1) * 512],
                               kc == 0, kc == 7, [wk, hk(kc, g)], [pk(b)])
                    if pas == 0:
                        for g in range(4):
                            ACT(sg[:, g, :], PS[g][:], AF.Silu, [pk(g)], [('sg', g)])
                    else:
                        for g in range(4):
                            TT('dve', act[:, jj, g * 512:(g + 1) * 512], sg[:, g, :], PS[4 + g][:], ALU.mult,
                               [('sg', g), pk(4 + g)], [('act', jj, g)])
            for n in range(8):
                wv, wk = ws.next()
                b0 = (n % 2) * 4
                for kc in range(11):
                    for g in range(4):
                        MM(PS[b0 + g][:], wv[:, kc, :], act[:, kc, g * 512:(g + 1) * 512], kc == 0, kc == 10,
                           [wk, ('act', kc, g)], [pk(b0 + g)])
                for g in range(4):
                    TT('dve', xT[:, n, g * 512:(g + 1) * 512], PS[b0 + g][:], xT[:, n, g * 512:(g + 1) * 512], ALU.add,
                       [pk(b0 + g), xk(n, g)], [xk(n, g)])

    def out_specs(w_d):
        wo = w_d.rearrange("(c p) n -> p c n", p=128)
        return [([(wo[:, :, nb * 256:(nb + 1) * 256], 0)], 8, 256) for nb in range(4)]

    def out_proj(ws, src, srckey, groups):
        ng = len(groups)
        for nb in range(4):
            wv, wk = ws.next()
            for cc in range(2):
                n = nb * 2 + cc
                bs = [bank() for _ in groups]
                for kc in range(8):
                    for gi, g in enumerate(groups):
                        MM(PS[bs[gi]][:], wv[:, kc, cc * 128:(cc + 1) * 128], src[:, kc, g * 512:(g + 1) * 512],
                           kc == 0, kc == 7, [wk, srckey(kc, g)], [pk(bs[gi])])
                for gi, g in enumerate(groups):
                    TT('dve', xT[:, n, g * 512:(g + 1) * 512], PS[bs[gi]][:], xT[:, n, g * 512:(g + 1) * 512], ALU.add,
                       [pk(bs[gi]), xk(n, g)], [xk(n, g)])

    def gla(l, j):
        rmsnorm(gattn, l)
        K1 = 1024
        qT = aview(0, [128, 4, 512], BF16)
        kT = aview(4 * K1, [128, 4, 512], BF16)
        ktok = aview(8 * K1, [128, 4, 512], BF16)
        vv = aview(12 * K1, [128, 4, 1024], BF16)
        gsr = aview(20 * K1, [128, 4, 1024], BF16)
        glrT = aview(28 * K1, [32, 512], BF16)
        Eb = aview(29 * K1, [128, 512], F32)
        la = aview(31 * K1, [128, 512], F32)
        ebT = aview(33 * K1, [128, 512], F32)
        enbT = aview(35 * K1, [128, 512], F32)
        es = aview(37 * K1, [128, 512], F32)
        qf = aview(39 * K1, [128, 4, 128], BF16)
        qc1 = aview(40 * K1, [128, 4, 128], BF16)
        qc2 = aview(41 * K1, [128, 4, 128], BF16)
        ktl = aview(42 * K1, [128, 4, 128], BF16)
        ks = aview(43 * K1, [128, 512], BF16)
        scT = aview(44 * K1, [128, 4, 128], BF16)
        S32 = aview(45 * K1, [128, 4, 256], F32)
        Sa = aview(49 * K1, [128, 4, 256], BF16)
        Sb = aview(51 * K1, [128, 4, 256], BF16)
        yy = aview(53 * K1, [128, 1024], BF16)
        junk = aview(55 * K1, [128, 256], BF16)
        ss4 = aview(55 * K1 + 512, [128, 4], F32)
        t4 = aview(55 * K1 + 544, [128, 4], F32)
        rs4 = aview(55 * K1 + 576, [128, 4], F32)
        gfull = aview(56 * K1, [128, 1024], BF16)
        W2b = aview(58 * K1, [32, 512], BF16)
        silt = aview(62 * K1, [128, 2, 256], BF16)
        ebTs = [ebT, aview(59 * K1, [128, 512], F32)]
        scTs = [scT, aview(61 * K1, [128, 4, 128], BF16)]
        kss = [ks, aview(63 * K1, [128, 512], BF16)]
        qc1s = [qc1, aview(64 * K1, [128, 4, 128], BF16)]
        qc2s = [qc2, aview(65 * K1, [128, 4, 128], BF16)]

        win = gla_w_in_d[j].rearrange("(c p) n -> p c n", p=128)
        DMA(W2s[0:16, :], gla_w2_d[j], 'w2s', w=['w2s'])
        DMA(W2s[16:17, :], gla_b_d[j:j + 1, :], 'w2s', r=['w2s'], w=['w2s'])
        CP('pool', W2b[0:17, :], W2s[0:17, :], ['w2s'], ['w2b'])
        DMA(grow[:], gla_ng_d[j:j + 1, :], 'grow', w=['grow'])
        b = bank()
        MM(PS[b][:, 0:256], ones_f[:], grow[:], True, True, ['grow'], [pk(b)], cr=['ones_f'])
        for h in range(4):
            CP('dve', gfull[:, h * 256:(h + 1) * 256], PS[b][:, 0:256], [pk(b)], [('gfull', h)])
        MS('pool', glrT[:], 1.0, ['glrT'])
        for p_ in range(2):
            MS('pool', qc1s[p_], 0.0, [('qc1', p_)])
            MS('pool', qc2s[p_], 0.0, [('qc2', p_)])
        MS('dve', S32[:], 0.0, [('S32', h) for h in range(4)])
        MS('pool', Sb[:], 0.0, ['Sb'])
        scale = 128 ** -0.5
        specs = []
        for qt in range(4):
            for blk in range(4):
                specs.append(([(win[:, :, blk * 256:(blk + 1) * 256], 0)], 8, 256))
            specs.append(([(win[:, :, 3072:3088], 0)], 8, 16))
            for cb in range(5):
                c0 = 512 + cb * 512
                specs.append(([(win[:, 0:4, c0:c0 + 512], 0)], 4, 512))
                specs.append(([(win[:, 4:8, c0:c0 + 512], 0)], 4, 512))
            specs += out_specs(gla_wo_d[j])
        ws = WStream(specs, ['dve', 'act'], look=1)
        silt5 = aview(62 * K1, [128, 512], BF16)

        for qt in range(4):
            t0q = qt * 512
            for blk in range(4):
                wv, wk = ws.next()
                for cc in range(2):
                    n = (blk % 2) * 2 + cc
                    b = bank()
                    for kc in range(8):
                        MM(PS[b][:], wv[:, kc, cc * 128:(cc + 1) * 128], hT[:, kc, t0q:t0q + 512], kc == 0, kc == 7,
                           [wk, hk(kc, qt)], [pk(b)])
                    dst = qT if blk < 2 else kT
                    CP('act' if cc == 0 else 'dve', dst[:, n, :], PS[b][:], [pk(b)], [('qT' if blk < 2 else 'kT', n)])
            wv, wk = ws.next()
            b = bank()
            for kc in range(8):
                MM(PS[b][0:16, :], wv[:, kc, :], hT[:, kc, t0q:t0q + 512], kc == 0, kc == 7, [wk, hk(kc, qt)], [pk(b)])
            CP('act', glrT[0:16, :], PS[b][0:16, :], [pk(b)], ['glrT'])
            for cb in range(5):
                wA, kA = ws.next()
                wB, kB = ws.next()
                for tt in range(4):
                    b = bank()
                    for kc in range(8):
                        wv_, wk_ = (wA, kA) if kc < 4 else (wB, kB)
                        MM(PS[b][:], hT[:, kc, t0q + tt * 128:t0q + (tt + 1) * 128], wv_[:, kc % 4, :],
                           kc == 0, kc == 7, [wk_, hk(kc, qt)], [pk(b)])
                    if cb == 0:
                        CP('dve', ktok[:, tt, :], PS[b][:], [pk(b)], [('ktok', tt)])
                    elif cb < 3:
                        vb = cb - 1
                        CP('act', vv[:, tt, vb * 512:(vb + 1) * 512], PS[b][:], [pk(b)], [('v', tt, 2 * vb), ('v', tt, 2 * vb + 1)])
                    else:
                        rb = cb - 3
                        ACT(silt5, PS[b][:], AF.Silu, [pk(b)], ['silt'])
                        TT('dve', gsr[:, tt, rb * 512:(rb + 1) * 512], silt5, gfull[:, rb * 512:(rb + 1) * 512], ALU.mult,
                           ['silt', ('gfull', 2 * rb), ('gfull', 2 * rb + 1)], [('gsr', tt, 2 * rb), ('gsr', tt, 2 * rb + 1)])
            def prepA_pe(tt):
                t0 = tt * 128
                MM(PS[0][:], glrT[0:17, t0:t0 + 128], W2b[0:17, :], True, True, ['glrT', 'w2b'], [pk(0)])

            def prepA_act(tt):
                ACT(Eb, PS[0][:], AF.Exp, [pk(0)], ['Eb'], scale=-1.0)
                ACT(la, Eb, AF.Ln, ['Eb'], ['la'], bias=onecol[:])

            def prepB_pe(tt):
                for h in range(4):
                    MM(PS[1][:, h * 128:(h + 1) * 128], la[:, h * 128:(h + 1) * 128], triS_f[:], True, True, ['la'], [pk(1)], cr=['triS_f'])
                MM(PS[0][:], triR_f[:], la, True, True, ['la'], [pk(0)], cr=['triR_f'])

            def prepB_act(tt):
                p = tt % 2
                ACT(ebTs[p], PS[1][:], AF.Exp, [pk(1)], [('ebT', p)])
                ACT(enbT, PS[1][:], AF.Exp, [pk(1)], ['enbT'], scale=-1.0)
                ACT(es, PS[0][:], AF.Exp, [pk(0)], ['es'])

            def prepB_dve(tt):
                t0 = tt * 128
                p = tt % 2
                STT(qf, qT[:, :, t0:t0 + 128], scale, ebTs[p].rearrange("p (h t) -> p h t", t=128), ALU.mult, ALU.mult,
                    [('qT', n) for n in range(4)] + [('ebT', p)], ['qf'])
                CP('pool', qc1s[p][:, :, 0:64], qf[:, :, 0:64], ['qf'], [('qc1', p)])
                CP('pool', qc2s[p][:, :, 64:128], qf[:, :, 64:128], ['qf'], [('qc2', p)])
                TT('dve', ktl, kT[:, :, t0:t0 + 128], enbT.rearrange("p (h t) -> p h t", t=128), ALU.mult,
                   [('kT', n) for n in range(4)] + ['enbT'], ['ktl'])
                TT('dve', kss[p], ktok[:, tt, :], es, ALU.mult, [('ktok', tt), 'es'], [('ks', p)])

            def prepC_pe(tt):
                for h in range(4):
                    MM(PS[3][:, h * 128:(h + 1) * 128], ktl[:, h, :], qf[:, h, :], True, True, ['ktl', 'qf'], [pk(3)])

            def prepC_dve(tt):
                p = tt % 2
                TT('dve', scTs[p], PS[3][:].rearrange("p (h t) -> p h t", t=128), maskG_b[:], ALU.mult, [pk(3)], [('scT', p)])

            def hv(h):
                return (4 + h // 2, slice((h % 2) * 256, (h % 2 + 1) * 256), 6 + h // 2,
                        slice((h % 2) * 256, (h % 2 + 1) * 256), slice(h * 256, (h + 1) * 256), slice(h * 128, (h + 1) * 128))

            def tile_step(tt, nx):
                p = tt % 2
                t0 = tt * 128
                ebT, ks, scT, qc1, qc2 = ebTs[p], kss[p], scTs[p], qc1s[p], qc2s[p]
                for h in range(4):
                    ob, osl, tb, tsl, vsl, ksl = hv(h)
                    MM(PS[ob][:, osl], scT[:, h, :], vv[:, tt, vsl], h % 2 == 0, False, [('scT', p), ('v', tt, h)], [pk(ob)])
                for h in range(4):
                    ob, osl, tb, tsl, vsl, ksl = hv(h)
                    MM(PS[tb][:, tsl], ks[0:64, ksl], vv[0:64, tt, vsl], True, True, [('ks', p), ('v', tt, h)], [pk(tb)])
                for h in range(4):
                    ob, osl, tb, tsl, vsl, ksl = hv(h)
                    MM(PS[ob][:, osl], qc1[:, h, :], Sb[:, h, :], False, False, [('qc1', p), 'Sb'], [pk(ob)])
                if nx:
                    prepA_pe(tt + 1)
                for h in range(4):
                    ob, osl, tb, tsl, vsl, ksl = hv(h)
                    STT(S32[:, h, :], S32[:, h, :], ebT[:, h * 128 + 63:h * 128 + 64], PS[tb][:, tsl], ALU.mult, ALU.add,
                        [('S32', h), ('ebT', p), pk(tb)], [('S32', h)])
                CP('act', Sa[:], S32[:], [('S32', h) for h in range(4)], ['Sa'])
                if nx:
                    prepA_act(tt + 1)
                for h in range(4):
                    ob, osl, tb, tsl, vsl, ksl = hv(h)
                    MM(PS[tb][:, tsl], ks[64:128, ksl], vv[64:128, tt, vsl], True, True, [('ks', p), ('v', tt, h)], [pk(tb)])
                for h in range(4):
                    ob, osl, tb, tsl, vsl, ksl = hv(h)
                    MM(PS[ob][:, osl], qc2[:, h, :], Sa[:, h, :], False, h % 2 == 1, [('qc2', p), 'Sa'], [pk(ob)])
                if nx:
                    prepB_pe(tt + 1)
                for h in range(4):
                    ob, osl, tb, tsl, vsl, ksl = hv(h)
                    STT(S32[:, h, :], S32[:, h, :], ebT[:, h * 128 + 127:h * 128 + 128], PS[tb][:, tsl], ALU.mult, ALU.add,
                        [('S32', h), ('ebT', p), pk(tb)], [('S32', h)])
                CP('act', Sb[:], S32[:], [('S32', h) for h in range(4)], ['Sb'])
                for h in range(4):
                    ob, osl, tb, tsl, vsl, ksl = hv(h)
                    ACT(junk, PS[ob][:, osl], AF.Square, [pk(ob)], ['junk', ('ss4', h)], accum_out=ss4[:, h:h + 1])
                ACT(t4, ss4, AF.Ln, [('ss4', h) for h in range(4)], ['t4'], scale=1.0 / 256.0, bias=epscol[:])
                ACT(rs4, t4, AF.Exp, ['t4'], ['rs4'], scale=-0.5)
                if nx:
                    prepB_act(tt + 1)
                for h in range(4):
                    ob, osl, tb, tsl, vsl, ksl = hv(h)
                    STT(yy[:, h * 256:(h + 1) * 256], PS[ob][:, osl], rs4[:, h:h + 1], gsr[:, tt, h * 256:(h + 1) * 256], ALU.mult, ALU.mult,
                        [pk(ob), 'rs4', ('gsr', tt, h)], [('yy', h)])
                ybank = PS[2][:].bitcast(BF16)
                for c in range(8):
                    TR(ybank[:, c * 128:(c + 1) * 128], yy[:, c * 128:(c + 1) * 128], ident_b[:], [('yy', c // 2)], [pk(2)])
                CP('act', hT[:, :, t0q + t0:t0q + t0 + 128], ybank.rearrange("p (c t) -> p c t", t=128), [pk(2)],
                   [hk(c, qt) for c in range(8)])
                if nx:
                    prepB_dve(tt + 1)
                    prepC_pe(tt + 1)
                    prepC_dve(tt + 1)

            prepA_pe(0)
            prepA_act(0)
            prepB_pe(0)
            prepB_act(0)
            prepB_dve(0)
            prepC_pe(0)
            prepC_dve(0)
            for tt in range(4):
                tile_step(tt, tt + 1 < 4)
            out_proj(ws, hT, hk, [qt])

    def sb(l, j):
        rmsnorm(gattn, l)
        K1 = 1024
        oT = aview(0, [128, 8, S], BF16)
        qTcs = [aview(32 * K1, [128, S], BF16), wst[2][:, 0:1024].bitcast(BF16)]
        kTcs = [aview(36 * K1, [128, S], BF16), wst[2][:, 1024:2048].bitcast(BF16)]
        XK = [('wst', 2, 0), ('wst', 2, 1)]
        vpad = aview(40 * K1, [128, 16, 2, 128], BF16)
        Ef = [aview(48 * K1 + i * 2048, [128, 512], F32) for i in range(2)]
        spb = [aview(52 * K1 + i * 1024, [128, 512], BF16) for i in range(2)] + [aview(64 * K1, [128, 512], BF16)]
        wTb = [aview(54 * K1 + i * 1024, [128, 512], BF16) for i in range(2)]
        acc32 = aview(56 * K1, [128, 512], F32)
        accb = [aview(58 * K1 + i * 1024, [128, 512], BF16) for i in range(2)]
        sqb = [aview(60 * K1 + i * 1024, [128, 512], BF16) for i in range(2)]
        lnt = aview(62 * K1, [128, 512], F32)
        win = sb_w_in_d[j].rearrange("(c p) n -> p c n", p=128)
        MS('pool', vpad[:], 0.0, [('vpad', tq_, h_) for tq_ in range(4) for h_ in range(2)])
        nrm = [0]
        specs = []
        for c in range(8):
            specs.append(([(win[:, :, c * 128:(c + 1) * 128], 0), (win[:, :, D + c * 128:D + (c + 1) * 128], 128)], 8, 256))
            specs.append(([(win[:, :, 2 * D + c * 128:2 * D + (c + 1) * 128], 0)], 8, 128))
        specs += out_specs(sb_wo_d[j])
        ws = WStream(specs, ['pool', 'dve'], look=1, nst=2)

        def qknorm(bs, dst, dkey, gvec):
            for g in range(4):
                i = nrm[0] % 2
                nrm[0] += 1
                ACT(sqb[i], PS[bs[g]][:], AF.Square, [pk(bs[g])], [('sqb', i)])
                cb = bank('C')
                MM(PS[cb][:], blk_b[:], sqb[i], True, True, [('sqb', i)], [pk(cb)], cr=['blk_b'])
                ACT(lnt, PS[cb][:], AF.Ln, [pk(cb)], ['lnt'], scale=1.0 / 64.0, bias=epscol[:])
                ACT(lnt, lnt, AF.Exp, ['lnt'], ['lnt'], scale=-0.5)
                STT(dst[:, g * 512:(g + 1) * 512], PS[bs[g]][:], gvec[:, j:j + 1], lnt, ALU.mult, ALU.mult,
                    [pk(bs[g]), 'lnt'], [dkey + (g,)])

        def make_units(wq, wqk_key, par):
            inj = {}
            for u in range(8):
                which, g = u // 4, u % 4
                base = 2 + u * 8
                dst = qTcs[par] if which == 0 else kTcs[par]
                dkey = ('qTc', par, g) if which == 0 else ('kTc', par, g)
                gvec = gq if which == 0 else gk
                i = u % 2

                def f_proj(which=which, g=g):
                    for kc in range(8):
                        MM(PS[6][:], wq[:, kc, which * 128:(which + 1) * 128], hT[:, kc, g * 512:(g + 1) * 512],
                           kc == 0, kc == 7, [wqk_key, hk(kc, g)], [pk(6)])

                def f_sq(i=i):
                    ACT(sqb[i], PS[6][:], AF.Square, [pk(6)], [('sqb', i)])

                def f_ss(i=i):
                    MM(PS[7][:], blk_b[:], sqb[i], True, True, [('sqb', i)], [pk(7)], cr=['blk_b'])

                def f_ln():
                    ACT(lnt, PS[7][:], AF.Ln, [pk(7)], ['lnt'], scale=1.0 / 64.0, bias=epscol[:])
                    ACT(lnt, lnt, AF.Exp, ['lnt'], ['lnt'], scale=-0.5)

                def f_stt(dst=dst, dkey=dkey, g=g, gvec=gvec):
                    STT(dst[:, g * 512:(g + 1) * 512], PS[6][:], gvec[:, j:j + 1], lnt, ALU.mult, ALU.mult,
                        [pk(6), 'lnt'], [dkey] + (XK if par == 1 else []))

                for off, f in enumerate([f_proj, f_sq, f_ss, f_ln, f_stt]):
                    inj.setdefault(base + off, []).append(f)
            return inj

        for c in range(8):
            par = c % 2
            qTc, kTc = qTcs[par], kTcs[par]

            def qkproj(which):
                bs = [0, 1, 2, 3]
                for kc in range(8):
                    for g in range(4):
                        MM(PS[bs[g]][:], wqk[:, kc, which * 128:(which + 1) * 128], hT[:, kc, g * 512:(g + 1) * 512],
                           kc == 0, kc == 7, [wqkk, hk(kc, g)], [pk(bs[g])])
                return bs

            def vproj(tqs):
                for tq in tqs:
                    b = 4 + tq % 2
                    for th in range(4):
                        tt = tq * 4 + th
                        for kc in range(8):
                            MM(PS[b][:, th * 128:(th + 1) * 128], hT[:, kc, tt * 128:(tt + 1) * 128], wvv[:, kc, :],
                               kc == 0, kc == 7, [wvk, hk(kc, tq)], [pk(b)])
                    pv = PS[b][:].rearrange("p (a n) -> p a n", n=128)
                    CP('dve', vpad[:, tq * 4:tq * 4 + 4, 0, 0:64], pv[:, :, 0:64], [pk(b)], [('vpad', tq, 0)])
                    CP('dve', vpad[:, tq * 4:tq * 4 + 4, 1, 64:128], pv[:, :, 64:128], [pk(b)], [('vpad', tq, 1)])

            if c == 0:
                wqk, wqkk = ws.next()
                bs = qkproj(0)
                wvv, wvk = ws.next()
                vproj([0, 1])
                qknorm(bs, qTc, ('qTc', par), gq)
                bs = qkproj(1)
                vproj([2, 3])
                qknorm(bs, kTc, ('kTc', par), gk)
            else:
                wvv, wvk = ws.next()
                vproj([0, 1, 2, 3])
            inj = {}
            if c < 7:
                wqn, wqnk = ws.next()
                inj = make_units(wqn, wqnk, 1 - par)
            tiles = []
            for qg in range(4):
                for h in range(2):
                    kbs = list(reversed(range(4 * qg + 4)))
                    for ti, kb in enumerate(kbs):
                        tiles.append(dict(qg=qg, h=h, kb=kb, first=(ti == 0), last=(ti == len(kbs) - 1),
                                          firstq=(h == 0 and ti == 0), lastq=(h == 1 and ti == len(kbs) - 1)))
            nt = len(tiles)
            obank = {}

            def stageA(i):
                t = tiles[i]
                qg, h, kb = t['qg'], t['h'], t['kb']
                di = kb - 4 * qg
                q0 = max(di, 0) * 128
                nq = 512 - q0
                t['q0'], t['nq'], t['di'] = q0, nq, di
                hp = slice(h * 64, (h + 1) * 64)
                zb = bank('A')
                t['zb'] = zb
                e = i % 2
                MM(PS[zb][:, 0:nq], kTc[hp, kb * 128:(kb + 1) * 128], qTc[hp, qg * 512 + q0:(qg + 1) * 512], True, True,
                   [('kTc', par, kb // 4), ('qTc', par, qg)] + (XK if par == 1 else []), [pk(zb)])
                ACT(Ef[e][:, 0:nq], PS[zb][:, 0:nq], AF.Exp, [pk(zb)], [('Ef', e)])

            def stageA2(i):
                t = tiles[i]
                nq, di = t['nq'], t['di']
                e = i % 2
                p3 = i % 3
                ACT(spb[p3][:, 0:nq], Ef[e][:, 0:nq], AF.Ln, [('Ef', e)], [('spb', p3)], bias=onecol[:])
                if di >= 0:
                    TT('pool', spb[p3][:, 0:128], spb[p3][:, 0:128], mask01_b[:], ALU.mult, [('spb', p3)], [('spb', p3)])

            def stageB(i):
                t = tiles[i]
                qg, h, kb, q0, nq, di, zb = t['qg'], t['h'], t['kb'], t['q0'], t['nq'], t['di'], t['zb']
                e = i % 2
                p3 = i % 3
                last_is_suffix = t['first'] and di < 0
                MM(PS[zb][:, 0:nq], negU_b[:], spb[p3][:, 0:nq], False, last_is_suffix, [('spb', p3)], [pk(zb)], cr=['negU_b'])
                if not t['first']:
                    MM(PS[zb][:, 0:nq], negO_b[:], accb[(i + 1) % 2][:, q0:512], False, di < 0, [('accb', (i + 1) % 2)], [pk(zb)], cr=['negO_b'])
                if di >= 0:
                    MM(PS[zb][:, 0:128], ident_b[:], negbig_b[:], False, True, [], [pk(zb)], cr=['ident_b', 'negbig_b'])
                ACT(wTb[e][:, 0:nq], PS[zb][:, 0:nq], AF.Exp, [pk(zb)], [('wTb', e)])
                if t['first']:
                    MS('dve', acc32, 0.0, ['acc32'])
                if not t['last']:
                    TT('dve', acc32[:, q0:512], acc32[:, q0:512], spb[p3][:, 0:nq], ALU.add, ['acc32', ('spb', p3)], ['acc32'])
                    CP('dve', accb[i % 2], acc32, ['acc32'], [('accb', i % 2)])

            def stageC(i):
                t = tiles[i]
                qg, h, kb, q0, nq = t['qg'], t['h'], t['kb'], t['q0'], t['nq']
                e = i % 2
                if t['firstq']:
                    ob = bank('B')
                    obank[qg] = ob
                    MM(PS[ob][:], zeros_b[:, 0:128], zeros_b[:], True, False, [], [pk(ob)], cr=['zeros_b'])
                ob = obank[qg]
                MM(PS[ob][:, q0:512], vpad[:, kb, h, :], wTb[e][:, 0:nq], False, t['lastq'], [('vpad', kb // 4, h), ('wTb', e)], [pk(ob)])
                if t['lastq']:
                    CP('dve', oT[:, c, qg * 512:(qg + 1) * 512], PS[ob][:], [pk(ob)], [('oT', c, qg)])

            for step in range(nt + 3):
                for f in inj.get(step, ()):
                    f()
                if step < nt:
                    stageA(step)
                if 0 <= step - 2 < nt:
                    stageB(step - 2)
                if step < nt:
                    stageA2(step)
                if 0 <= step - 3 < nt:
                    stageC(step - 3)
        out_proj(ws, oT, lambda kc, g: ('oT', kc, g), [0, 1, 2, 3])

    finals = []
    for s in range(nseq):
        if s == 0:
            swap_x(None, 0)
        for l in layers:
            if l % 2 == 0:
                gla(l, l // 2)
            else:
                sb(l, l // 2)
            ffn(l)
        finals += swap_x(s, s + 1 if s + 1 < nseq else None)
    P.emit(final_wait_ops=finals)
    return nc, P


_CACHE = {}


def kernel(**inputs):
    x = np.ascontiguousarray(inputs['x'], dtype=np.float32)
    B = x.shape[0]
    nseq = B // NCORES
    key = (nseq,)
    if key not in _CACHE:
        _CACHE[key] = build(nseq, list(range(DEPTH)))[0]
    nc = _CACHE[key]
    names = ['attn_norm_g', 'ffn_norm_g', 'gla_w_in', 'gla_w_gate2', 'gla_b_gate', 'gla_norm_g', 'gla_w_out',
             'sb_w_in', 'sb_q_norm_g', 'sb_k_norm_g', 'sb_w_out', 'ffn_w_gate_up', 'ffn_w_down']
    shared = {n: np.ascontiguousarray(inputs[n], dtype=np.float32) for n in names}
    in_maps = []
    for c in range(NCORES):
        m = dict(shared)
        m['x'] = x[c * nseq:(c + 1) * nseq]
        in_maps.append(m)
    res = run_bass_kernel_spmd(nc, in_maps, core_ids=list(range(NCORES)))
    return np.concatenate([r['y'] for r in res.results], axis=0).astype(np.float32)
```
